# Optimizing a Trainium2 kernel written in Bass

```python
import jax, jax.numpy as jnp
from jax import lax
import numpy as np

D_MODEL = 2048
BATCH = 4
SEQ = 4096
DEPTH = 2

CHUNK = 128
A_GROUPS = 8
A_WIDTH = 1024
A_GROUP_DIM = A_WIDTH // A_GROUPS
B_HEADS = 8
B_HEAD_DIM = 128
B_WIDTH = B_HEADS * B_HEAD_DIM
Q_BLOCK = 128
EVEN_IN = 2 * A_WIDTH + 3 * B_WIDTH + B_HEADS
EVEN_OUT = A_WIDTH + B_WIDTH
C_HEAD_DIM = 64
C_HEADS = D_MODEL // C_HEAD_DIM
LORA_W = 96
LORA_A = 96
LORA_G = 256
N_MIX = 6
N_EXPERTS = 16
N_GROUPS = 4
EXPERTS_PER_GROUP = N_EXPERTS // N_GROUPS
TOP_K = 2
D_EXPERT = 1024
ALPHA = (2 * DEPTH) ** 0.25
BETA = (8 * DEPTH) ** -0.25
LN_EPS = 1e-5
GN_EPS = 64e-5
N_EVEN = (DEPTH + 1) // 2
N_ODD = DEPTH // 2

kernel_name = 'hybrid_sgu_fox_rwkv7_grouped_moe_deepnorm'


def layer_norm(x, g, b, eps=LN_EPS):
    xf = x.astype(jnp.float32)
    mu = jnp.mean(xf, -1, keepdims=True)
    var = jnp.mean(jnp.square(xf - mu), -1, keepdims=True)
    return ((xf - mu) * lax.rsqrt(var + eps)).astype(x.dtype) * g + b


def spatial_gating(z, w_s, b_s, g_v, b_v):
    Bsz, T, _ = z.shape
    u, v = jnp.split(z, 2, axis=-1)
    v = layer_norm(v, g_v, b_v)
    v = v.reshape(Bsz, T // CHUNK, CHUNK, A_GROUPS, A_GROUP_DIM)
    causal = jnp.tril(jnp.ones((CHUNK, CHUNK), dtype=bool))
    w = jnp.where(causal, w_s, 0)
    s = jnp.einsum('gts,bcsgd->bctgd', w, v) + b_s.T[None, None, :, :, None]
    return u * s.reshape(Bsz, T, A_WIDTH)


def forgetting_attention(q, k, v, f_logit):
    Bsz, T, H, Dh = q.shape
    log_f = jax.nn.log_sigmoid(f_logit.astype(jnp.float32))
    c = jnp.cumsum(log_f, axis=1).transpose(0, 2, 1)
    scale = Dh ** -0.5
    n_blocks = T // Q_BLOCK
    qb = q.reshape(Bsz, n_blocks, Q_BLOCK, H, Dh).transpose(1, 0, 3, 2, 4)
    cb = c.reshape(Bsz, H, n_blocks, Q_BLOCK).transpose(2, 0, 1, 3)
    k_pos = jnp.arange(T)

    def block(args):
        i, q_i, c_i = args
        s = jnp.einsum('bhqd,bkhd->bhqk', q_i, k).astype(jnp.float32) * scale
        s = s + c_i[..., :, None] - c[:, :, None, :]
        q_pos = i * Q_BLOCK + jnp.arange(Q_BLOCK)
        s = jnp.where(q_pos[:, None] >= k_pos[None, :], s, -jnp.inf)
        p = jax.nn.softmax(s, axis=-1).astype(v.dtype)
        return jnp.einsum('bhqk,bkhd->bqhd', p, v)

    out = lax.map(block, (jnp.arange(n_blocks), qb, cb))
    return out.transpose(1, 0, 2, 3, 4).reshape(Bsz, T, H * Dh)


def even_mixer(x, w_in, b_a, w_s, b_s, g_v, b_v, b_f, w_out):
    Bsz, T, _ = x.shape
    proj = x @ w_in
    o1 = 2 * A_WIDTH
    z_a, q, k, v, f_logit = jnp.split(proj, [o1, o1 + B_WIDTH, o1 + 2 * B_WIDTH, o1 + 3 * B_WIDTH], axis=-1)
    y_a = spatial_gating(jax.nn.gelu(z_a + b_a), w_s, b_s, g_v, b_v)
    heads = lambda t: t.reshape(Bsz, T, B_HEADS, B_HEAD_DIM)
    y_b = forgetting_attention(heads(q), heads(k), heads(v), f_logit + b_f)
    return jnp.concatenate([y_a, y_b], axis=-1) @ w_out


def rwkv7_step(S, inp):
    r_t, w_t, k_t, v_t, a_t, b_t = inp
    sa = jnp.einsum('bhvk,bhk->bhv', S, a_t)
    S = S * w_t[:, :, None, :] + sa[..., None] * b_t[:, :, None, :] + v_t[..., None] * k_t[:, :, None, :]
    return S, jnp.einsum('bhvk,bhk->bhv', S, r_t)


def rwkv7_time_mix(x, mu, w_rkv, w0, w1, w2, a0, a1, a2, g1, g2, k_k, k_a, r_k, gn_g, gn_b, w_o):
    Bsz, T, D = x.shape
    H, N = C_HEADS, C_HEAD_DIM
    f32 = jnp.float32
    x_prev = jnp.pad(x, ((0, 0), (1, 0), (0, 0)))[:, :-1]
    xm = x[None] + (x_prev - x)[None] * mu[:, None, None, :]
    r, k, v = jnp.einsum('nbtd,nde->nbte', xm[:3], w_rkv)
    w = -jax.nn.softplus(-(w0 + jnp.tanh(xm[3] @ w1) @ w2)) - 0.5
    a = jax.nn.sigmoid(a0 + (xm[4] @ a1) @ a2)
    g = jax.nn.sigmoid(xm[5] @ g1) @ g2
    heads = lambda t: t.reshape(Bsz, T, H, N)
    kk = heads(k * k_k).astype(f32)
    kk = kk / jnp.maximum(jnp.linalg.norm(kk, axis=-1, keepdims=True), 1e-12)
    k = k * (1 + (a - 1) * k_a)
    decay = jnp.exp(-jnp.exp(heads(w).astype(f32)))
    r_h, k_h, v_h, a_h = heads(r), heads(k), heads(v), heads(a)
    seq = tuple(jnp.moveaxis(t.astype(f32), 1, 0) for t in (r_h, decay, k_h, v_h, -kk, kk * a_h))
    S0 = jnp.zeros((Bsz, H, N, N), f32)
    _, y = lax.scan(rwkv7_step, S0, seq)
    y = jnp.moveaxis(y, 0, 1)
    y = layer_norm(y, gn_g.reshape(H, N), gn_b.reshape(H, N), GN_EPS)
    bonus = jnp.sum(r_h.astype(f32) * k_h.astype(f32) * r_k, axis=-1, keepdims=True) * v_h.astype(f32)
    y = (y + bonus).astype(x.dtype).reshape(Bsz, T, D) * g
    return y @ w_o


def grouped_moe(h, w_router, b_router, w_gu, w_down):
    Bsz, T, D = h.shape
    t = h.reshape(-1, D)
    logits = (t @ w_router).astype(jnp.float32) + b_router
    probs = jax.nn.softmax(logits, axis=-1)
    grouped = probs.reshape(-1, N_GROUPS, EXPERTS_PER_GROUP)
    group_score = jnp.sum(lax.top_k(grouped, TOP_K)[0], axis=-1)
    sel_group = jnp.argmax(group_score, axis=-1)
    in_group = (jnp.arange(N_EXPERTS) // EXPERTS_PER_GROUP)[None, :] == sel_group[:, None]
    top_p, top_i = lax.top_k(jnp.where(in_group, probs, -1.0), TOP_K)
    top_p = top_p / jnp.sum(top_p, axis=-1, keepdims=True)
    gates = jnp.sum(jax.nn.one_hot(top_i, N_EXPERTS, dtype=jnp.float32) * top_p[..., None], axis=1).astype(h.dtype)
    y = jnp.zeros_like(t)
    for e in range(N_EXPERTS):
        g_e, u_e = jnp.split(t @ w_gu[e], 2, axis=-1)
        y = y + (gates[:, e:e + 1] * jax.nn.silu(g_e) * u_e) @ w_down[e]
    return y.reshape(Bsz, T, D)


def setup_inputs(seed: int = 0) -> dict:
    key = jax.random.key(seed)
    ks = iter(jax.random.split(key, 64))
    D = D_MODEL
    nrm = lambda shape, scale: jax.random.normal(next(ks), shape, jnp.float32) * scale
    uni = lambda shape, lo, hi: jax.random.uniform(next(ks), shape, jnp.float32, lo, hi)
    return {
        'x': nrm((BATCH, SEQ, D), 1.0),
        'ev_w_in': nrm((N_EVEN, D, EVEN_IN), D ** -0.5),
        'ev_b_a': nrm((N_EVEN, 2 * A_WIDTH), 0.02),
        'ev_w_s': nrm((N_EVEN, A_GROUPS, CHUNK, CHUNK), CHUNK ** -0.5),
        'ev_b_s': 1.0 + nrm((N_EVEN, A_GROUPS, CHUNK), 0.1),
        'ev_g_v': 1.0 + nrm((N_EVEN, A_WIDTH), 0.05),
        'ev_b_v': nrm((N_EVEN, A_WIDTH), 0.02),
        'ev_b_f': 4.0 + nrm((N_EVEN, B_HEADS), 0.5),
        'ev_w_out': nrm((N_EVEN, EVEN_OUT, D), BETA * EVEN_OUT ** -0.5),
        'rw_mu': uni((N_ODD, N_MIX, D), 0.0, 1.0),
        'rw_w_rkv': nrm((N_ODD, 3, D, D), D ** -0.5),
        'rw_w0': uni((N_ODD, D), -6.0, -1.0),
        'rw_w1': nrm((N_ODD, D, LORA_W), D ** -0.5),
        'rw_w2': nrm((N_ODD, LORA_W, D), 0.1 * LORA_W ** -0.5),
        'rw_a0': nrm((N_ODD, D), 0.1),
        'rw_a1': nrm((N_ODD, D, LORA_A), D ** -0.5),
        'rw_a2': nrm((N_ODD, LORA_A, D), 0.1 * LORA_A ** -0.5),
        'rw_g1': nrm((N_ODD, D, LORA_G), D ** -0.5),
        'rw_g2': nrm((N_ODD, LORA_G, D), LORA_G ** -0.5),
        'rw_k_k': 0.85 + nrm((N_ODD, D), 0.02),
        'rw_k_a': 1.0 + nrm((N_ODD, D), 0.02),
        'rw_r_k': nrm((N_ODD, C_HEADS, C_HEAD_DIM), 0.1),
        'rw_gn_g': 1.0 + nrm((N_ODD, D), 0.05),
        'rw_gn_b': nrm((N_ODD, D), 0.02),
        'rw_w_o': nrm((N_ODD, D, D), BETA * D ** -0.5),
        'ln_g': 1.0 + nrm((DEPTH, 2, D), 0.05),
        'ln_b': nrm((DEPTH, 2, D), 0.02),
        'w_router': nrm((D, N_EXPERTS), D ** -0.5),
        'b_router': nrm((N_EXPERTS,), 0.01),
        'w_gu': nrm((DEPTH, N_EXPERTS, D, 2 * D_EXPERT), D ** -0.5),
        'w_down': nrm((DEPTH, N_EXPERTS, D_EXPERT, D), BETA * D_EXPERT ** -0.5),
    }


def reference(x, ev_w_in, ev_b_a, ev_w_s, ev_b_s, ev_g_v, ev_b_v, ev_b_f, ev_w_out,
              rw_mu, rw_w_rkv, rw_w0, rw_w1, rw_w2, rw_a0, rw_a1, rw_a2, rw_g1, rw_g2,
              rw_k_k, rw_k_a, rw_r_k, rw_gn_g, rw_gn_b, rw_w_o,
              ln_g, ln_b, w_router, b_router, w_gu, w_down):
    for layer in range(DEPTH):
        i = layer // 2
        if layer % 2 == 0:
            mixed = even_mixer(x, ev_w_in[i], ev_b_a[i], ev_w_s[i], ev_b_s[i], ev_g_v[i], ev_b_v[i],
                               ev_b_f[i], ev_w_out[i])
        else:
            mixed = rwkv7_time_mix(x, rw_mu[i], rw_w_rkv[i], rw_w0[i], rw_w1[i], rw_w2[i], rw_a0[i],
                                   rw_a1[i], rw_a2[i], rw_g1[i], rw_g2[i], rw_k_k[i], rw_k_a[i],
                                   rw_r_k[i], rw_gn_g[i], rw_gn_b[i], rw_w_o[i])
        x = layer_norm(ALPHA * x + mixed, ln_g[layer, 0], ln_b[layer, 0])
        x = layer_norm(ALPHA * x + grouped_moe(x, w_router, b_router, w_gu[layer], w_down[layer]),
                       ln_g[layer, 1], ln_b[layer, 1])
    return x
```

```python
import numpy as np
import concourse.bass as bass
import concourse.mybir as mybir
from concourse.bass_utils import run_bass_kernel_spmd

F32 = mybir.dt.float32
BF16 = mybir.dt.bfloat16
I32 = mybir.dt.int32
AF = mybir.ActivationFunctionType
ALU = mybir.AluOpType
AX = mybir.AxisListType

PE, ACT, DVE, POOL, SP = "tensor", "scalar", "vector", "gpsimd", "sync"
ENGS = [PE, ACT, DVE, POOL, SP]


class Op:
    __slots__ = ("eng", "emit", "deps", "is_dma", "key", "sig", "idx")

    def __init__(self, eng, emit, is_dma, key):
        self.eng = eng
        self.emit = emit
        self.deps = []
        self.is_dma = is_dma
        self.key = key
        self.sig = None
        self.idx = None


class Sched:
    def __init__(self, nc):
        self.nc = nc
        self.ops = []
        self.last_w = {}
        self.readers = {}
        self.last_by_key = {}
        self.barrier_deps = []

    def join(self, resources):
        last = None
        for op in reversed(self.ops):
            if op.is_dma:
                last = op
                break
        for r in resources:
            self.last_w[r] = last

    def barrier(self):
        self.barrier_deps = list(self.last_by_key.values())

    def add(self, eng, emit, reads=(), writes=(), dma=False, key=None):
        if dma:
            key = ("dma", key if key is not None else (tuple(writes)[0] if len(writes) else tuple(reads)[0]))
        op = Op(eng, emit, dma, key)
        deps = []
        for r in list(reads) + list(writes):
            w = self.last_w.get(r)
            if w is not None:
                deps.append(w)
        for w in writes:
            deps.extend(self.readers.get(w, ()))
        deps.extend(self.barrier_deps)
        seen = set()
        for d in deps:
            if id(d) in seen:
                continue
            seen.add(id(d))
            if d.eng == PE and eng == PE and not d.is_dma and not dma:
                continue
            op.deps.append(d)
        for w in writes:
            self.last_w[w] = op
            self.readers[w] = []
        for r in reads:
            self.readers.setdefault(r, []).append(op)
        self.ops.append(op)
        self.last_by_key[key if dma else ("eng", eng)] = op
        return op

    def mm(self, out, lhsT, rhs, start, stop, reads, writes, **kw):
        return self.add(PE, lambda e: e.matmul(out, lhsT, rhs, start=start, stop=stop, **kw), reads, writes)

    def tr(self, out, in_, ident, reads, writes):
        return self.add(PE, lambda e: e.transpose(out, in_, ident), reads, writes)

    def act(self, out, in_, func, reads, writes, **kw):
        return self.add(ACT, lambda e: e.activation(out, in_, func, **kw), reads, writes)

    def tt(self, eng, out, in0, in1, op, reads, writes):
        return self.add(eng, lambda e: e.tensor_tensor(out, in0, in1, op), reads, writes)

    def ts(self, eng, out, in0, s1, s2, op0, op1, reads, writes, **kw):
        return self.add(eng, lambda e: e.tensor_scalar(out, in0, s1, s2, op0, op1, **kw), reads, writes)

    def cp(self, eng, out, in_, reads, writes):
        if eng == ACT:
            return self.add(ACT, lambda e: e.copy(out, in_), reads, writes)
        return self.add(eng, lambda e: e.tensor_copy(out, in_), reads, writes)

    def dma(self, eng, out, in_, reads, writes, key=None, **kw):
        if eng == POOL and "max_dma_last_dim" not in kw:
            kw["max_dma_last_dim"] = 4096
        return self.add(eng, lambda e: e.dma_start(out=out, in_=in_, **kw), reads, writes, dma=True, key=key)

    def finalize(self, final_wait_keys=()):
        nc = self.nc
        for op in self.ops:
            if op.is_dma:
                op.sig = True
            for d in op.deps:
                d.sig = True
        final_ops = []
        for r in final_wait_keys:
            w = self.last_w.get(r)
            if w is not None:
                w.sig = True
                final_ops.append(w)
        sem_names = {}
        counts = {}
        for op in self.ops:
            if op.sig:
                k = op.key if op.is_dma else ("eng", op.eng)
                sem_names.setdefault(k, len(sem_names))
                counts[k] = counts.get(k, 0) + 1
                op.idx = counts[k]
        self.n_sems = len(sem_names)
        per_eng = {e: [] for e in ENGS}
        for op in self.ops:
            per_eng[op.eng].append(op)
        from contextlib import ExitStack
        with ExitStack() as st:
            sems = {}
            for k, i in sem_names.items():
                sems[k] = st.enter_context(nc.semaphore("s%d" % i))
            block = st.enter_context(nc.Block())

            def run(eng_name, e):
                waited = {}
                for op in per_eng[eng_name]:
                    need = {}
                    for d in op.deps:
                        k = d.key if d.is_dma else ("eng", d.eng)
                        v = d.idx * (16 if d.is_dma else 1)
                        if need.get(k, 0) < v:
                            need[k] = v
                    for k, v in need.items():
                        if waited.get(k, 0) >= v:
                            continue
                        e.wait_ge(sems[k], v)
                        waited[k] = v
                    ins = op.emit(e)
                    if op.sig:
                        k = op.key if op.is_dma else ("eng", op.eng)
                        ins.then_inc(sems[k], 16 if op.is_dma else 1)
                if eng_name == SP:
                    for w in final_ops:
                        k = w.key if w.is_dma else ("eng", w.eng)
                        v = w.idx * (16 if w.is_dma else 1)
                        e.wait_ge(sems[k], v)

            @block.tensor
            def _(e):
                run(PE, e)

            @block.scalar
            def _(e):
                run(ACT, e)

            @block.vector
            def _(e):
                run(DVE, e)

            @block.gpsimd
            def _(e):
                run(POOL, e)

            @block.sync
            def _(e):
                run(SP, e)

import ml_dtypes

from contextlib import ExitStack

D = 2048
NE = 16
DE = 1024
ALPHA = 4 ** 0.25
LN_EPS = 1e-5
NWGU = 2
NWD = 2
import os
DEBUG = int(os.environ.get('TAIL_DEBUG', '0'))
CUT = int(os.environ.get('TAIL_CUT', '9'))


class Ctx:
    def __init__(self, nc):
        self.nc = nc
        self.n = 0

    def sb(self, st, shape, dt, name=None):
        self.n += 1
        return st.enter_context(self.nc.sbuf_tensor("sb_%s_%d" % (name or "t", self.n), shape, dt))

    def ps(self, st, shape, dt, name=None):
        self.n += 1
        return st.enter_context(self.nc.psum_tensor("ps_%s_%d" % (name or "p", self.n), shape, dt))


def make_ident(S, C, st):
    identf = C.sb(st, [128, 128], F32, "identf")
    identb = C.sb(st, [128, 128], BF16, "identb")
    S.add(POOL, lambda e: e.memset(identf[:], 0.0), [], ["identf"])
    S.add(POOL, lambda e: e.affine_select(identf[:], identf[:], [[-1, 128]], ALU.not_equal, 1.0, base=0,
                                          channel_multiplier=1), ["identf"], ["identf"])
    S.cp(DVE, identb[:], identf[:], ["identf"], ["identb"])
    return identf, identb


def emit_ln(S, C, src, dst, gbc, bbc, tmp, reads, writes, eps, tag, ncol=2048, xn_eng=DVE, aff_eng=POOL):
    nch = ncol // 512
    st6, mv, sq, rstd = tmp["st6"], tmp["mv"], tmp["sq"], tmp["rstd"]
    r_st = tag + "_st"
    for c in range(nch):
        S.add(DVE, lambda e, c=c: e.bn_stats(st6[:, c, :], src[:, c * 512:(c + 1) * 512]), reads, [(r_st, c)])
    S.add(DVE, lambda e: e.bn_aggr(mv[:], st6[:, 0:nch, :].rearrange("p c s -> p (c s)")),
          [(r_st, c) for c in range(nch)], [tag + "_mv"])
    S.act(sq[:], mv[:, 1:2], AF.Sqrt, [tag + "_mv", "epsc"], [tag + "_sq"], bias=tmp["epsc"][:], scale=1.0)
    S.add(DVE, lambda e: e.reciprocal(rstd[:], sq[:]), [tag + "_sq"], [tag + "_rstd"])
    S.ts(xn_eng, src, src, mv[:, 0:1], rstd[:], ALU.subtract, ALU.mult, list(reads) + [tag + "_mv", tag + "_rstd"],
         list(reads))
    S.tt(aff_eng, src, src, gbc, ALU.mult, list(reads) + ["lnconst"], list(reads))
    S.tt(aff_eng, dst, src, bbc, ALU.add, list(reads) + ["lnconst"], writes)


def emit_tail(S, C, nc, NT, PASS, dr, lay, identf, identb):
    NB = NT // 128
    TTW = min(512, PASS)
    with ExitStack() as st0:
        gates = C.sb(st0, [128, NB, 16], F32, "gates")
        epsc = C.sb(st0, [128, 1], F32, "epsc")
        S.add(POOL, lambda e: e.memset(epsc[:], LN_EPS), [], ["epsc"])
        lng = C.sb(st0, [128, 2, 2048], F32, "lng")
        lnb = C.sb(st0, [128, 2, 2048], F32, "lnb")
        for i in range(2):
            S.dma(SP, lng[:, i, :], dr["lng"][i].partition_broadcast(128), [], ["lnconst"], key=("lnc", i))
            S.dma(SP, lnb[:, i, :], dr["lnb"][i].partition_broadcast(128), [], ["lnconst"], key=("lnc", 2 + i))
        lntmp = [dict(st6=C.sb(st0, [128, 4, 6], F32), mv=C.sb(st0, [128, 2], F32), sq=C.sb(st0, [128, 1], F32),
                      rstd=C.sb(st0, [128, 1], F32), epsc=epsc) for _ in range(2)]
        with ExitStack() as st:
            wo = C.sb(st, [128, 16, 2048], BF16, "wo")
            for h in range(4):
                S.dma(POOL, wo[:, 4 * h:4 * h + 4, :], dr["wo"][:, 4 * h:4 * h + 4, :], [], [("wo", h)])
            wr = C.sb(st, [128, 16, 16], F32, "wr")
            S.dma(SP, wr[:], dr["wr"], [], ["wr"])
            brb = C.sb(st, [128, 16], F32, "brb")
            S.dma(SP, brb[:], dr["br"].partition_broadcast(128), [], ["brb"])
            yTt = [C.sb(st, [128, 16, 128], BF16) for _ in range(2)]
            xr = [C.sb(st, [128, 2048], F32) for _ in range(2)]
            xT32 = [C.sb(st, [128, 16, 128], F32) for _ in range(2)]
            xT16 = [C.sb(st, [128, 16, 128], BF16) for _ in range(2)]
            pmix = [C.ps(st, [128, 2048], F32) for _ in range(1)]
            ptr = [C.ps(st, [128, 4, 128], F32) for _ in range(2)]
            plogf = C.ps(st, [128, 512], F32, "plog")
            plog = plogf[:, 0:16]
            rt = {k: C.sb(st, shp, F32, "rt_" + k) for k, shp in
                  dict(l=[128, 16], m=[128, 1], e=[128, 16], g1=[128, 4], mk=[128, 16], e2=[128, 16], g2=[128, 4],
                       gs=[128, 4], gm=[128, 1], gk=[128, 4], s1=[128, 16], rg=[128, 1]).items()}
            for b in range(NB):
                i = b % 2
                R = lambda n: (n, i)
                S.dma(SP, yTt[i][:], dr["yT"][:, :, b * 128:(b + 1) * 128], [], [R("yTt")])
                S.dma(SP, xr[i][:], dr["xres"][b * 128:(b + 1) * 128, :], [], [R("xr")])
                pm = pmix[0]
                for cb in range(4):
                    for k in range(16):
                        S.mm(pm[:, cb * 512:(cb + 1) * 512], yTt[i][:, k, :], wo[:, k, cb * 512:(cb + 1) * 512],
                             k == 0, k == 15, [R("yTt"), ("wo", k // 4)], [("pmix", cb)])
                for cb in range(4):
                    S.add(DVE, lambda e, cb=cb, i=i: e.scalar_tensor_tensor(
                        xr[i][:, cb * 512:(cb + 1) * 512], xr[i][:, cb * 512:(cb + 1) * 512], ALPHA,
                        pm[:, cb * 512:(cb + 1) * 512], ALU.mult, ALU.add), [R("xr"), ("pmix", cb)], [R("xr")])
                if CUT >= 2:
                    emit_ln(S, C, xr[i][:], xr[i][:], lng[:, 0, :], lnb[:, 0, :], lntmp[i], [R("xr")], [R("xr")], LN_EPS,
                            "ln1_%d" % i)
                S.dma(SP, dr["x1s"][b * 128:(b + 1) * 128, :], xr[i][:], [R("xr")], [("x1s", b)], key=R("xr_st"))
                if DEBUG == 1:
                    S.dma(SP, dr["out"][b * 128:(b + 1) * 128, :], xr[i][:], [R("xr")], [("out", b)], key=R("xr_st2"))
                if CUT < 3:
                    continue
                for q in range(4):
                    pt = ptr[q % 2]
                    for k in range(4 * q, 4 * q + 4):
                        S.tr(pt[:, k - 4 * q, :], xr[i][:, k * 128:(k + 1) * 128], identf[:], [R("xr"), "identf"],
                             [("ptr", q % 2)])
                    S.cp(ACT, xT32[i][:, 4 * q:4 * q + 4, :], pt[:], [("ptr", q % 2)], [R("xT32")])
                    S.cp(DVE, xT16[i][:, 4 * q:4 * q + 4, :], xT32[i][:, 4 * q:4 * q + 4, :], [R("xT32")], [R("xT16")])
                S.dma(SP, dr["x1T"][:, :, b * 128:(b + 1) * 128], xT16[i][:], [R("xT16")], [("x1T", b)], key=R("xT16_st"))
                if CUT < 4:
                    continue
                for k in range(16):
                    S.mm(plog, xT32[i][:, k, :], wr[:, k, :], k == 0, k == 15, [R("xT32"), "wr"], ["plog"])
                t = rt
                S.tt(DVE, t["l"][:], plog, brb[:], ALU.add, ["plog", "brb"], ["rt_l"])
                S.add(DVE, lambda e: e.tensor_reduce(t["m"][:], t["l"][:], AX.X, ALU.max), ["rt_l"], ["rt_m"])
                S.ts(DVE, t["m"][:], t["m"][:], -1.0, None, ALU.mult, ALU.bypass, ["rt_m"], ["rt_m"])
                S.act(t["e"][:], t["l"][:], AF.Exp, ["rt_l", "rt_m"], ["rt_e"], bias=t["m"][:], scale=1.0)
                e3 = t["e"][:].rearrange("p (g e) -> p g e", g=4)
                S.add(DVE, lambda e: e.tensor_reduce(t["g1"][:], e3, AX.X, ALU.max), ["rt_e"], ["rt_g1"])
                S.tt(DVE, t["mk"][:].rearrange("p (g e) -> p g e", g=4), e3,
                     t["g1"][:].unsqueeze(2).to_broadcast([128, 4, 4]), ALU.is_equal, ["rt_e", "rt_g1"], ["rt_mk"])
                S.add(DVE, lambda e: e.scalar_tensor_tensor(t["e2"][:], t["mk"][:], -1e30, t["e"][:], ALU.mult,
                                                            ALU.add), ["rt_mk", "rt_e"], ["rt_e2"])
                S.add(DVE, lambda e: e.tensor_reduce(t["g2"][:], t["e2"][:].rearrange("p (g e) -> p g e", g=4), AX.X,
                                                     ALU.max), ["rt_e2"], ["rt_g2"])
                S.tt(DVE, t["gs"][:], t["g1"][:], t["g2"][:], ALU.add, ["rt_g1", "rt_g2"], ["rt_gs"])
                S.add(DVE, lambda e: e.tensor_reduce(t["gm"][:], t["gs"][:], AX.X, ALU.max), ["rt_gs"], ["rt_gm"])
                S.ts(DVE, t["gk"][:], t["gs"][:], t["gm"][:], None, ALU.is_equal, ALU.bypass, ["rt_gs", "rt_gm"],
                     ["rt_gk"])
                S.tt(DVE, t["s1"][:].rearrange("p (g e) -> p g e", g=4), e3,
                     t["g2"][:].unsqueeze(2).to_broadcast([128, 4, 4]), ALU.is_ge, ["rt_e", "rt_g2"], ["rt_s1"])
                S.tt(DVE, t["s1"][:].rearrange("p (g e) -> p g e", g=4),
                     t["s1"][:].rearrange("p (g e) -> p g e", g=4),
                     t["gk"][:].unsqueeze(2).to_broadcast([128, 4, 4]), ALU.mult, ["rt_s1", "rt_gk"], ["rt_s1"])
                S.add(DVE, lambda e: e.reciprocal(t["rg"][:], t["gm"][:]), ["rt_gm"], ["rt_rg"])
                S.add(DVE, lambda e, b=b: e.scalar_tensor_tensor(gates[:, b, :], t["s1"][:], t["rg"][:], t["e"][:],
                                                                 ALU.mult, ALU.mult), ["rt_s1", "rt_rg", "rt_e"],
                      [("gates", b)])
        if DEBUG == 1:
            return
        S.barrier()
        NP = NT // PASS
        NBP = PASS // 128
        NTT = PASS // TTW
        with ExitStack() as st:
            xT = C.sb(st, [128, 16, PASS], BF16, "moe_xT")
            yacc = C.sb(st, [128, NBP, 2048], F32, "yacc")
            hb = [C.sb(st, [128, 8, PASS], BF16) for _ in range(2)]
            wgu = [C.sb(st, [128, 16, 2, 128], BF16) for _ in range(NWGU)]
            wd = [C.sb(st, [128, 8, 512], BF16) for _ in range(NWD)]
            sg = [C.sb(st, [128, TTW], F32) for _ in range(2)]
            pg = [C.ps(st, [128, 512], F32)[:, 0:TTW] for _ in range(2)]
            pu = [C.ps(st, [128, 512], F32)[:, 0:TTW] for _ in range(2)]
            py = [C.ps(st, [128, 512], F32) for _ in range(2)]
            xo = [C.sb(st, [128, 2048], F32) for _ in range(1)]
            ngu = 0
            nd = 0
            nps = 0
            npy = 0
            for p in range(NP):
                for k4 in range(4):
                    S.dma(SP, xT[:, 4 * k4:4 * k4 + 4, :], dr["x1T"][:, 4 * k4:4 * k4 + 4, p * PASS:(p + 1) * PASS],
                          [("x1T", b) for b in range(p * NBP, (p + 1) * NBP)], [("moe_xT", k4)])
                for ex in range(NE):
                    hi = ex % 2
                    for f in range(8):
                        wi = ngu % NWGU
                        ngu += 1
                        S.dma(POOL, wgu[wi][:], dr["wgu"][ex, f], [], [("wgu", wi)])
                        for tt in range(NTT):
                            pi = nps % 2
                            nps += 1
                            for k in range(16):
                                S.mm(pg[pi], wgu[wi][:, k, 0, :], xT[:, k, tt * TTW:(tt + 1) * TTW], k == 0, k == 15,
                                     [("wgu", wi), ("moe_xT", k // 4)], [("pg", pi)])
                            for k in range(16):
                                S.mm(pu[pi], wgu[wi][:, k, 1, :], xT[:, k, tt * TTW:(tt + 1) * TTW], k == 0, k == 15,
                                     [("wgu", wi), ("moe_xT", k // 4)], [("pu", pi)])
                            S.act(sg[pi][:], pg[pi], AF.Silu, [("pg", pi)], [("sg", pi)])
                            S.tt(DVE, hb[hi][:, f, tt * TTW:(tt + 1) * TTW], sg[pi][:], pu[pi], ALU.mult,
                                 [("sg", pi), ("pu", pi)], [("hb", hi, f)])
                    for cb in range(4):
                        di = nd % NWD
                        nd += 1
                        S.dma(POOL, wd[di][:], dr["wd"][ex, cb], [], [("wd", di)])
                        for bb in range(NBP):
                            b = p * NBP + bb
                            yi = npy % 2
                            npy += 1
                            for k in range(8):
                                S.mm(py[yi][:], hb[hi][:, k, bb * 128:(bb + 1) * 128], wd[di][:, k, :], k == 0, k == 7,
                                     [("hb", hi, k), ("wd", di)], [("py", yi)])
                            ysl = yacc[:, bb, cb * 512:(cb + 1) * 512]
                            if ex == 0:
                                S.ts(DVE, ysl, py[yi][:], gates[:, b, ex:ex + 1], None, ALU.mult, ALU.bypass,
                                     [("py", yi), ("gates", b)], [("yacc", bb, cb)])
                            else:
                                S.add(DVE, lambda e, ysl=ysl, yi=yi, b=b, ex=ex: e.scalar_tensor_tensor(
                                    ysl, py[yi][:], gates[:, b, ex:ex + 1], ysl, ALU.mult, ALU.add),
                                      [("py", yi), ("gates", b), ("yacc", bb, cb)], [("yacc", bb, cb)])
                for bb in range(NBP):
                    b = p * NBP + bb
                    i = 0
                    S.dma(SP, xo[i][:], dr["x1s"][b * 128:(b + 1) * 128, :], [("x1s", b)], [("xo", i)])
                    S.add(DVE, lambda e, i=i, bb=bb: e.scalar_tensor_tensor(
                        xo[i][:], xo[i][:], ALPHA, yacc[:, bb, :], ALU.mult, ALU.add),
                          [("xo", i)] + [("yacc", bb, cb) for cb in range(4)], [("xo", i)])
                    emit_ln(S, C, xo[i][:], xo[i][:], lng[:, 1, :], lnb[:, 1, :], lntmp[i], [("xo", i)], [("xo", i)],
                            LN_EPS, "ln2_%d" % i)
                    S.dma(SP, dr["out"][b * 128:(b + 1) * 128, :], xo[i][:], [("xo", i)], [("out", b)],
                          key=("xo_st", i))


SCALE = 128 ** -0.5
NEG = -30000.0


def emit_gelu(S, src_ps, bias_ap, bias_is_col, dst, tmp, rd, wr, tag):
    z, a = tmp["z"], tmp["a"]
    rz, ra = (tag, "z"), (tag, "a")
    if bias_is_col:
        S.act(z, src_ps, AF.Identity, rd, [rz], bias=bias_ap, scale=1.0)
    else:
        S.tt(DVE, z, src_ps, bias_ap, ALU.add, rd, [rz])
    S.act(a, z, AF.Square, [rz], [ra])
    S.ts(DVE, a, a, 0.044715, 1.0, ALU.mult, ALU.add, [ra], [ra])
    S.tt(DVE, a, a, z, ALU.mult, [ra, rz], [ra])
    S.act(a, a, AF.Sigmoid, [ra], [ra], scale=1.5957691216)
    S.tt(DVE, dst, a, z, ALU.mult, [ra, rz], wr)


def emit_front0(S, C, nc, NBA, dr, identf, identb):
    NBO = NBA // 2
    TA = NBA * 128
    NT = NBO * 128
    TW = min(512, NT)
    with ExitStack() as st0:
        ctok = C.sb(st0, [128, NBA, 8], F32, "ctok")
        nctok = C.sb(st0, [128, NBA, 8], F32, "nctok")
        chl = C.sb(st0, [40, TA], BF16, "chl")
        epsc = C.sb(st0, [128, 1], F32, "epsc0")
        S.add(POOL, lambda e: e.memset(epsc[:], LN_EPS), [], ["epsc"])
        S.add(POOL, lambda e: e.memset(chl[:], 0.0), [], ["chl"])
        lf = C.sb(st0, [128, NBA, 8], F32, "lf")
        with ExitStack() as st:
            xT = C.sb(st, [128, 16, NT], BF16, "xT")
            xl = [C.sb(st, [128, 2048], F32) for _ in range(2)]
            xb = [C.sb(st, [128, 2048], BF16) for _ in range(2)]
            ptp = [C.ps(st, [128, 8, 128], BF16) for _ in range(2)]
            wt = [C.sb(st, [128, 16, 128], BF16) for _ in range(3)]
            pj = [C.ps(st, [128, 512], F32) for _ in range(3)]
            gt = [dict(z=C.sb(st, [128, 512], F32), a=C.sb(st, [128, 512], F32)) for _ in range(2)]
            bau = C.sb(st, [128, 8], F32, "bau")
            S.dma(SP, bau[:], dr["bau"], [], ["bau"])
            ost = [C.sb(st, [128, 512], BF16) for _ in range(2)]
            wtm = C.sb(st, [128, 16, 1024], BF16, "wtm")
            bav = C.sb(st, [128, 1024], F32, "bav")
            gv = C.sb(st, [128, 1024], F32, "gv")
            bv = C.sb(st, [128, 1024], F32, "bv")
            S.dma(SP, bav[:], dr["bav"].partition_broadcast(128), [], ["bav"])
            S.dma(SP, gv[:], dr["gv"].partition_broadcast(128), [], ["lnconst"], key="gv")
            S.dma(SP, bv[:], dr["bv"].partition_broadcast(128), [], ["lnconst"], key="bv")
            vtile = [C.sb(st, [128, 1024], F32) for _ in range(2)]
            vst = [C.sb(st, [128, 1024], BF16) for _ in range(2)]
            lnt = [dict(st6=C.sb(st, [128, 4, 6], F32), mv=C.sb(st, [128, 2], F32), sq=C.sb(st, [128, 1], F32),
                        rstd=C.sb(st, [128, 1], F32), epsc=epsc) for _ in range(2)]
            wf = C.sb(st, [128, 16, 8], BF16, "wf")
            S.dma(POOL, wf[:], dr["wf"], [], ["wf"])
            bfb = C.sb(st, [128, 8], F32, "bfb")
            S.dma(SP, bfb[:], dr["bf"].partition_broadcast(128), [], ["bfb"])
            nw = 0
            npj = 0
            no = 0
            for hp in range(2):
                for bl in range(NBO):
                    b = hp * NBO + bl
                    i = b % 2
                    S.dma(SP, xl[i][:], dr["xall"][b * 128:(b + 1) * 128, :], [], [("xl", i)])
                    S.cp(ACT, xb[i][:], xl[i][:], [("xl", i)], [("xb", i)])
                    for hh in range(2):
                        for k in range(8):
                            kk = hh * 8 + k
                            S.tr(ptp[hh][:, k, :], xb[i][:, kk * 128:(kk + 1) * 128], identb[:], [("xb", i), "identb"],
                                 [("ptp", hh)])
                        S.cp(DVE, xT[:, hh * 8:(hh + 1) * 8, bl * 128:(bl + 1) * 128], ptp[hh][:], [("ptp", hh)],
                             [("xT", bl)])
                for kind in (("u", "q", "k") if hp == 0 else ("k",)):
                    for j in range(8):
                        wi = nw % 3
                        nw += 1
                        S.dma(POOL, wt[wi][:], dr["w" + kind][j], [], [("wt", wi)])
                        for t0 in range(0, NT, TW):
                            pi = npj % 3
                            npj += 1
                            oi = no % 2
                            no += 1
                            blks = [("xT", bb) for bb in range(t0 // 128, (t0 + TW) // 128)]
                            for k in range(16):
                                S.mm(pj[pi][:, 0:TW], wt[wi][:, k, :], xT[:, k, t0:t0 + TW], k == 0, k == 15,
                                     [("wt", wi)] + blks, [("pj", pi)])
                            if kind == "u":
                                g = gt[npj % 2]
                                emit_gelu(S, pj[pi][:, 0:TW], bau[:, j:j + 1], True, ost[oi][:, 0:TW],
                                          dict(z=g["z"][:, 0:TW], a=g["a"][:, 0:TW]), [("pj", pi), "bau"],
                                          [("ost", oi)], ("gt", npj % 2))
                                S.dma(SP, dr["uTd"][:, j, t0:t0 + TW], ost[oi][:, 0:TW], [("ost", oi)], [("uTd", j)],
                                      key=("ost_st", oi))
                            elif kind == "q":
                                S.act(ost[oi][:, 0:TW], pj[pi][:, 0:TW], AF.Copy, [("pj", pi)], [("ost", oi)],
                                      scale=SCALE)
                                S.dma(SP, dr["qTd"][:, j, t0:t0 + TW], ost[oi][:, 0:TW], [("ost", oi)], [("qTd", j)],
                                      key=("ost_st", oi))
                            else:
                                S.cp(ACT, ost[oi][:, 0:TW], pj[pi][:, 0:TW], [("pj", pi)], [("ost", oi)])
                                S.dma(SP, dr["kT"][:, j, hp * NT + t0:hp * NT + t0 + TW], ost[oi][:, 0:TW],
                                      [("ost", oi)], [("kT", j)], key=("ost_st", oi))
                if hp == 0:
                    for ch in range(2):
                        S.dma(POOL, wtm[:, :, ch * 512:(ch + 1) * 512], dr["wvs"][ch], [], [("wtm", ch)])
                    for bl in range(NBO):
                        i = bl % 2
                        for ch in range(2):
                            pi = npj % 3
                            npj += 1
                            for k in range(16):
                                S.mm(pj[pi][:], xT[:, k, bl * 128:(bl + 1) * 128], wtm[:, k, ch * 512:(ch + 1) * 512],
                                     k == 0, k == 15, [("xT", bl), ("wtm", ch)], [("pj", pi)])
                            g = gt[npj % 2]
                            emit_gelu(S, pj[pi][:], bav[:, ch * 512:(ch + 1) * 512], False,
                                      vtile[i][:, ch * 512:(ch + 1) * 512], dict(z=g["z"][:], a=g["a"][:]),
                                      [("pj", pi), "bav"], [("vtile", i)], ("gt", npj % 2))
                        emit_ln(S, C, vtile[i][:], vst[i][:], gv[:], bv[:], lnt[i], [("vtile", i)], [("vst", i)],
                                LN_EPS, "lnv_%d" % i, ncol=1024)
                        S.dma(SP, dr["vsd"][bl * 128:(bl + 1) * 128, :], vst[i][:], [("vst", i)], [("vsd", bl)],
                              key=("vst_st", i))
                for ch in range(2):
                    S.dma(POOL, wtm[:, :, ch * 512:(ch + 1) * 512], dr["wv"][ch], [], [("wtm", ch)])
                for bl in range(NBO):
                    b = hp * NBO + bl
                    i = bl % 2
                    for ch in range(2):
                        pi = npj % 3
                        npj += 1
                        for k in range(16):
                            S.mm(pj[pi][:], xT[:, k, bl * 128:(bl + 1) * 128], wtm[:, k, ch * 512:(ch + 1) * 512],
                                 k == 0, k == 15, [("xT", bl), ("wtm", ch)], [("pj", pi)])
                        S.cp(ACT if ch == 0 else DVE, vst[i][:, ch * 512:(ch + 1) * 512], pj[pi][:], [("pj", pi)],
                             [("vst", i)])
                    S.dma(SP, dr["V"][b * 128:(b + 1) * 128, :], vst[i][:], [("vst", i)], [("V", b)],
                          key=("vst_st", i))
                for bl in range(NBO):
                    b = hp * NBO + bl
                    pi = npj % 3
                    npj += 1
                    for k in range(16):
                        S.mm(pj[pi][:, 0:8], xT[:, k, bl * 128:(bl + 1) * 128], wf[:, k, :], k == 0, k == 15,
                             [("xT", bl), "wf"], [("pj", pi)])
                    S.tt(DVE, lf[:, b, :], pj[pi][:, 0:8], bfb[:], ALU.add, [("pj", pi), "bfb"], [("lf", b)])
        S.barrier()
        with ExitStack() as st:
            pj = [C.ps(st, [128, 512], F32) for _ in range(1)]
            lfall = [("lf", b) for b in range(NBA)]
            lf2 = lf[:].rearrange("p b h -> p (b h)")
            S.act(lf2, lf2, AF.Exp, lfall, lfall, scale=-1.0)
            S.act(lf2, lf2, AF.Ln, lfall, lfall, bias=1.0, scale=1.0)
            S.ts(DVE, lf2, lf2, -1.0, None, ALU.mult, ALU.bypass, lfall, lfall)
            utri = C.sb(st, [128, 128], F32, "utri")
            ones = C.sb(st, [128, 128], F32, "onesf")
            S.add(POOL, lambda e: e.memset(ones[:], 1.0), [], ["onesf"])
            S.add(POOL, lambda e: e.memset(utri[:], 1.0), [], ["utri"])
            S.add(POOL, lambda e: e.affine_select(utri[:], utri[:], [[1, 128]], ALU.is_ge, 0.0, base=0,
                                                  channel_multiplier=-1), ["utri"], ["utri"])
            hf = C.sb(st, [128, 2], F32, "hf")
            S.dma(SP, hf[:], dr["hf"], [], ["hf"])
            pc = C.ps(st, [128, 512], F32, "pc")
            ptot = C.ps(st, [128, 512], F32, "ptot")
            tot = C.sb(st, [128, 2, 8], F32, "tot")
            for half in range(2):
                for j in range(NBO):
                    S.mm(ptot[:, half * 8:half * 8 + 8], ones[:], lf[:, half * NBO + j, :], j == 0, j == NBO - 1,
                         ["onesf"] + lfall, ["ptot"])
            for half in range(2):
                S.cp(DVE, tot[:, half, :], ptot[:, half * 8:half * 8 + 8], ["ptot"], [("tot", half)])
            S.ts(DVE, tot[:, 1, :], tot[:, 1, :], hf[:, 0:1], None, ALU.mult, ALU.bypass, [("tot", 1), "hf"],
                 [("tot", 1)])
            S.ts(DVE, tot[:, 0, :], tot[:, 0, :], hf[:, 1:2], None, ALU.mult, ALU.bypass, [("tot", 0), "hf"],
                 [("tot", 0)])
            for half in range(2):
                for j in range(NBO):
                    b = half * NBO + j
                    for i2 in range(j + 1):
                        S.mm(pc[:, 0:8], utri[:] if i2 == j else ones[:], lf[:, half * NBO + i2, :], i2 == 0, i2 == j,
                             ["utri", "onesf"] + lfall, ["pc"])
                    S.tt(DVE, ctok[:, b, :], pc[:, 0:8], tot[:, 1 - half, :], ALU.add, ["pc", ("tot", 1 - half)],
                         [("ctok", b)])
            call = [("ctok", b) for b in range(NBA)]
            S.ts(DVE, nctok[:].rearrange("p b h -> p (b h)"), ctok[:].rearrange("p b h -> p (b h)"), -1.0, None,
                 ALU.mult, ALU.bypass, call, ["nctok"])
            pct = C.ps(st, [8, 512], F32, "pct")
            c32 = C.sb(st, [8, 512], F32, "c32")
            chi = C.sb(st, [8, 512], BF16, "chi")
            chf = C.sb(st, [8, 512], F32, "chf")
            clo = C.sb(st, [8, 512], BF16, "clo")
            for b0 in range(0, NBA, 4):
                nb = min(4, NBA - b0)
                for bb in range(nb):
                    S.tr(pct[:, bb * 128:(bb + 1) * 128], ctok[:, b0 + bb, :], identf[:], [("ctok", b0 + bb), "identf"],
                         ["pct"])
                w = nb * 128
                S.cp(DVE, c32[:, 0:w], pct[:, 0:w], ["pct"], ["c32"])
                S.cp(DVE, chl[0:8, b0 * 128:b0 * 128 + w], c32[:, 0:w], ["c32"], ["chl"])
                S.cp(DVE, chf[:, 0:w], chl[0:8, b0 * 128:b0 * 128 + w], ["chl"], ["chf"])
                S.tt(DVE, c32[:, 0:w], c32[:, 0:w], chf[:, 0:w], ALU.subtract, ["c32", "chf"], ["c32"])
                S.cp(DVE, clo[:, 0:w], c32[:, 0:w], ["c32"], ["clo"])
                S.dma(SP, chl[32:40, b0 * 128:b0 * 128 + w], clo[:, 0:w], ["clo"], ["chl"], key="clo_mv")
        S.barrier()
        with ExitStack() as st:
            uT = C.sb(st, [128, 8, NT], BF16, "uT")
            vsb = C.sb(st, [128, NBO, 1024], BF16, "vsb")
            for j in range(8):
                S.dma(SP, uT[:, j, :], dr["uTd"][:, j, :], [("uTd", j)], [("uT", j)], key="ld_uT")
            S.join([("uT", j) for j in range(8)])
            for b in range(NBO):
                S.dma(SP, vsb[:, b, :], dr["vsd"][b * 128:(b + 1) * 128, :], [("vsd", b)], [("vsb", b)], key="ld_vsb")
            S.join([("vsb", b) for b in range(NBO)])
            wsT = C.sb(st, [128, 8, 128], BF16, "wsT")
            S.dma(POOL, wsT[:], dr["wsT"], [], ["wsT"])
            for g in range(8):
                S.add(POOL, lambda e, g=g: e.affine_select(wsT[:, g, :], wsT[:, g, :], [[1, 128]], ALU.is_ge, 0.0,
                                                           base=0, channel_multiplier=-1), ["wsT"], ["wsT"])
            bsb = C.sb(st, [128, 8, 128], F32, "bsb")
            S.dma(SP, bsb[:].rearrange("p g t -> p (g t)"), dr["bs"].partition_broadcast(128), [], ["bsb"])
            psg = [C.ps(st, [128, 512], F32) for _ in range(2)]
            t1 = [C.sb(st, [128, 512], F32) for _ in range(2)]
            yo = [C.sb(st, [128, 512], BF16) for _ in range(2)]
            n = 0
            NG4 = TW // 128
            for g in range(8):
                for b0 in range(0, NBO, NG4):
                    i = n % 2
                    n += 1
                    for bb in range(NG4):
                        b = b0 + bb
                        S.mm(psg[i][:, bb * 128:(bb + 1) * 128], vsb[:, b, g * 128:(g + 1) * 128], wsT[:, g, :], True,
                             True, [("vsb", b), "wsT"], [("psg", i)])
                    for bb in range(NG4):
                        S.tt(DVE, t1[i][:, bb * 128:(bb + 1) * 128], psg[i][:, bb * 128:(bb + 1) * 128], bsb[:, g, :],
                             ALU.add, [("psg", i), "bsb"], [("t1", i)])
                    S.tt(DVE, yo[i][:, 0:TW], t1[i][:, 0:TW], uT[:, g, b0 * 128:b0 * 128 + TW], ALU.mult,
                         [("t1", i), ("uT", g)], [("yo", i)])
                    S.dma(SP, dr["yT"][:, g, b0 * 128:b0 * 128 + TW], yo[i][:, 0:TW], [("yo", i)],
                          [("yT", g, b0)], key=("yo_st", i))
        S.barrier()
        with ExitStack() as st:
            qT = C.sb(st, [128, 8, NT], BF16, "qT")
            for j in range(8):
                S.dma(SP, qT[:, j, :], dr["qTd"][:, j, :], [("qTd", j)], [("qT", j)], key="ld_qT")
            S.join([("qT", j) for j in range(8)])
            kT = C.sb(st, [128, 8, TA], BF16, "kTs")
            Vs = C.sb(st, [128, NBA, 1024], BF16, "Vs")
            for j in range(8):
                S.dma(SP, kT[:, j, :], dr["kT"][:, j, :], [("kT", j)], [("kTs", j)], key="ld_kTs")
            S.join([("kTs", j) for j in range(8)])
            for b in range(NBA):
                S.dma(SP, Vs[:, b, :], dr["V"][b * 128:(b + 1) * 128, :], [("V", b)], [("Vs", b)], key="ld_Vs")
            S.join([("Vs", b) for b in range(NBA)])
            sel = C.sb(st, [40, 8, 128], BF16, "sel")
            S.add(POOL, lambda e: e.memset(sel[:], 0.0), [], ["sel"])
            for off in (0, 32):
                S.add(POOL, lambda e, off=off: e.affine_select(sel[:], sel[:], [[-1, 8], [0, 128]], ALU.not_equal, 1.0,
                                                                 base=-off, channel_multiplier=1), ["sel"], ["sel"])
            onesb = C.sb(st, [128, 128], BF16, "onesb")
            S.add(POOL, lambda e: e.memset(onesb[:], 1.0), [], ["onesb"])
            mk = [C.sb(st, [128, NBA, 128], BF16) for _ in range(2)]
            pS = [C.ps(st, [128, 512], F32) for _ in range(3)]
            pO = [C.ps(st, [128, 512], F32) for _ in range(2)]
            pL = [C.ps(st, [128, 512], F32) for _ in range(2)]
            sS = [C.sb(st, [128, 512], F32) for _ in range(2)]
            pT = [C.sb(st, [128, 512], BF16) for _ in range(2)]
            rl = [C.sb(st, [128, 128], F32) for _ in range(2)]
            ob = [C.sb(st, [128, 128], BF16) for _ in range(2)]
            nS = 0
            nQ = 0
            for s in range(NBO):
                mi = s % 2
                S.dma(SP, mk[mi][:], dr["maskT"][s], [], [("mk", mi)])
                for hd in range(8):
                    oi = nQ % 2
                    nQ += 1
                    for c0 in range(0, NBA, 4):
                        nb = min(4, NBA - c0)
                        si = nS % 3
                        bi = nS % 2
                        nS += 1
                        for bb in range(nb):
                            kb = c0 + bb
                            S.mm(pS[si][:, bb * 128:(bb + 1) * 128], kT[:, hd, kb * 128:(kb + 1) * 128],
                                 qT[:, hd, s * 128:(s + 1) * 128], True, False, [("kTs", hd), ("qT", hd)],
                                 [("pS", si)])
                            S.mm(pS[si][:, bb * 128:(bb + 1) * 128], sel[:, hd, :], chl[:, s * 128:(s + 1) * 128],
                                 False, True, ["sel", "chl"], [("pS", si)])
                        for bb in range(nb):
                            kb = c0 + bb
                            S.add(DVE, lambda e, bi=bi, si=si, bb=bb, kb=kb, hd=hd, mi=mi: e.scalar_tensor_tensor(
                                sS[bi][:, bb * 128:(bb + 1) * 128], pS[si][:, bb * 128:(bb + 1) * 128],
                                nctok[:, kb, hd:hd + 1], mk[mi][:, kb, :], ALU.add, ALU.add),
                                  [("pS", si), "nctok", ("mk", mi)], [("sS", bi)])
                        w = nb * 128
                        S.act(pT[bi][:, 0:w], sS[bi][:, 0:w], AF.Exp, [("sS", bi)], [("pT", bi)])
                        for bb in range(nb):
                            kb = c0 + bb
                            S.mm(pO[oi][:, 0:128], Vs[:, kb, hd * 128:(hd + 1) * 128], pT[bi][:, bb * 128:(bb + 1) * 128],
                                 kb == 0, kb == NBA - 1, [("Vs", kb), ("pT", bi)], [("pO", oi)])
                            S.mm(pL[oi][:, 0:128], onesb[:], pT[bi][:, bb * 128:(bb + 1) * 128], kb == 0,
                                 kb == NBA - 1, ["onesb", ("pT", bi)], [("pL", oi)])
                    S.add(DVE, lambda e, oi=oi: e.reciprocal(rl[oi][:], pL[oi][:, 0:128]), [("pL", oi)], [("rl", oi)])
                    S.tt(DVE, ob[oi][:], pO[oi][:, 0:128], rl[oi][:], ALU.mult, [("pO", oi), ("rl", oi)], [("ob", oi)])
                    S.dma(SP, dr["yT"][:, 8 + hd, s * 128:(s + 1) * 128], ob[oi][:], [("ob", oi)], [("yT", 8 + hd, s)],
                          key=("ob_st", oi))
    S.barrier()


GN_EPS = 64e-5
RW_TW = int(os.environ.get('RW_TW', '512'))
RW_STOP = int(os.environ.get('RW_STOP', '9'))
RW_A1 = int(os.environ.get('RW_A1', '9'))
RW_NB = int(os.environ.get('RW_NB', '2'))
RW_A0 = int(os.environ.get('RW_A0', '9'))
RW_SUB = int(os.environ.get('RW_SUB', '9'))
RW_HP = int(os.environ.get('RW_HP', '2'))
EW = 0.6065306597126334


def emit_rwkv(S, C, nc, T, NCT, dr, identf, identb):
    NCHK = T // 64
    TW = min(RW_TW, T)
    NTT = T // TW
    CPT = TW // 64
    GC = 4
    NH = 2 * NCT
    with ExitStack() as st0:
        m_su = C.sb(st0, [64, 64], BF16, "m_su")
        m_ui = C.sb(st0, [64, 64], BF16, "m_ui")
        m_sl = C.sb(st0, [64, 64], BF16, "m_sl")
        for m, cm, base in ((m_su, -1, -1), (m_ui, -1, 0)):
            S.add(POOL, lambda e, m=m: e.memset(m[:], 1.0), [], ["masks"])
            S.add(POOL, lambda e, m=m, cm=cm, base=base: e.affine_select(m[:], m[:], [[1, 64]], ALU.is_ge, 0.0,
                                                                         base=base, channel_multiplier=cm), ["masks"],
                  ["masks"])
        S.add(POOL, lambda e: e.memset(m_sl[:], 1.0), [], ["masks"])
        S.add(POOL, lambda e: e.affine_select(m_sl[:], m_sl[:], [[-1, 64]], ALU.is_ge, 0.0, base=-1,
                                              channel_multiplier=1), ["masks"], ["masks"])
        bones = C.sb(st0, [128, 128], F32, "bones")
        S.add(POOL, lambda e: e.memset(bones[:], 0.0), [], ["bones"])
        S.add(POOL, lambda e: e.memset(bones[0:64, 0:64], 1.0), ["bones"], ["bones"])
        S.add(POOL, lambda e: e.memset(bones[64:128, 64:128], 1.0), ["bones"], ["bones"])
        bonesb = C.sb(st0, [128, 128], BF16, "bonesb")
        S.cp(DVE, bonesb[:], bones[:], ["bones"], ["bonesb"])
        smask = C.sb(st0, [128, TW], F32, "smask")
        S.add(POOL, lambda e: e.memset(smask[:], 1.0), [], ["smask"])
        S.add(POOL, lambda e: e.memset(smask[:].rearrange("p (c t) -> p c t", t=64)[:, :, 0:1], 0.0), ["smask"],
              ["smask"])
        cv = {}
        for nm in ("w0", "a0", "kk", "ka", "rk", "gng", "gnb"):
            cv[nm] = C.sb(st0, [128, NCT], F32, "cv_" + nm)
            S.dma(SP, cv[nm][:], dr[nm], [], [("cvr", nm)], key="ld_cv")
        S.join([("cvr", nm) for nm in ("w0", "a0", "kk", "ka", "rk", "gng", "gnb")] + ["cv"])
        mu = C.sb(st0, [128, 6, 16], F32, "mu")
        S.dma(SP, mu[:], dr["mu"], [], ["mu"])
        epsg = C.sb(st0, [128, 1], F32, "epsg")
        S.add(POOL, lambda e: e.memset(epsg[:], GN_EPS), [], ["epsg"])
        tiny = C.sb(st0, [128, 1], F32, "tiny")
        S.add(POOL, lambda e: e.memset(tiny[:], 1e-24), [], ["tiny"])
        with ExitStack() as st:
            wst = {n: [C.sb(st, [128, 16, 128], BF16, "W%s%d" % (n, i)) for i in range(2)] for n in ("r", "k", "v")}
            w1 = C.sb(st, [128, 16, 96], BF16, "w1")
            a1 = C.sb(st, [128, 16, 96], BF16, "a1")
            g1 = C.sb(st, [128, 16, 256], BF16, "g1")
            w2 = C.sb(st, [96, NCT * 128], BF16, "w2")
            a2 = C.sb(st, [96, NCT * 128], BF16, "a2")
            g2 = C.sb(st, [128, 2, NCT * 128], BF16, "g2")
            for t_, nm in ((w1, "w1"), (a1, "a1"), (g1, "g1"), (w2, "w2"), (a2, "a2"), (g2, "g2")):
                S.dma(POOL, t_[:], dr[nm], [], [nm], key="ld_lora")
            S.join(["w1", "a1", "g1", "w2", "a2", "g2"])
            xl = [C.sb(st, [128, 2048], F32) for _ in range(1)]
            xb = [C.sb(st, [128, 2048], BF16) for _ in range(1)]
            xT = C.sb(st, [128, 16, TW + 1], BF16, "xT")
            dx = C.sb(st, [128, 16, TW], BF16, "dx")
            xm = [C.sb(st, [128, 16, TW], BF16) for _ in range(2)]
            S.add(POOL, lambda e: e.memset(xT[:, :, 0:1], 0.0), [], ["xTprev"])
            ptp = [C.ps(st, [128, 8, 128], BF16) for _ in range(2)]
            pj = [C.ps(st, [128, 512], F32) for _ in range(3)]
            plo = C.ps(st, [128, 512], F32, "plo")
            hlo = {n: C.sb(st, [128, TW], BF16, "h" + n) for n in ("w", "a")}
            hg = C.sb(st, [128, 2, TW], BF16, "hg")
            F = lambda nm: [C.sb(st, [128, TW], F32, "%s%d" % (nm, i)) for i in range(2)]
            tr_, tk_, tv_, ta_, tb_, tld, tlc, tt1, tt2 = F("tr"), F("tk"), F("tv"), F("ta"), F("tb"), F("tld"), F("tlc"), F("tt1"), F("tt2")
            tkk, tkm = F("tkk"), F("tkm")
            ob = {n: [C.sb(st, [128, TW], BF16, "o%s%d" % (n, i)) for i in range(2)] for n in
                  ("B", "K", "Bh", "Kh", "v", "g")}
            oar = [C.sb(st, [128, CPT, 128], BF16, "oar%d" % i) for i in range(2)]
            wend = [C.sb(st, [128, CPT], F32, "wend%d" % i) for i in range(2)]
            npj = 0
            nu = 0
            for tt in range(NTT):
                t0 = tt * TW
                if tt > 0:
                    S.cp(DVE, xT[:, :, 0:1], xT[:, :, TW:TW + 1], [("xTt", b) for b in range(TW // 128)], ["xTprev"])
                for bl in range(TW // 128):
                    b = tt * (TW // 128) + bl
                    i = 0
                    S.dma(SP, xl[i][:], dr["x"][b * 128:(b + 1) * 128, :], [], [("xl", i)])
                    S.cp(ACT, xb[i][:], xl[i][:], [("xl", i)], [("xb", i)])
                    for hh in range(2):
                        for k in range(8):
                            kk_ = hh * 8 + k
                            S.tr(ptp[hh][:, k, :], xb[i][:, kk_ * 128:(kk_ + 1) * 128], identb[:], [("xb", i), "identb"],
                                 [("ptp", hh)])
                        S.cp(DVE, xT[:, hh * 8:(hh + 1) * 8, 1 + bl * 128:1 + (bl + 1) * 128], ptp[hh][:],
                             [("ptp", hh)], [("xTt", bl)])
                xall = [("xTt", b) for b in range(TW // 128)] + ["xTprev"]
                S.tt(DVE, dx[:], xT[:, :, 0:TW], xT[:, :, 1:TW + 1], ALU.subtract, xall, ["dx"])

                def mix(n, slot):
                    for k in range(16):
                        S.add(DVE, lambda e, k=k: e.scalar_tensor_tensor(xm[slot][:, k, :], dx[:, k, :], mu[:, n, k:k + 1],
                                                                         xT[:, k, 1:TW + 1], ALU.mult, ALU.add),
                              ["dx", "mu"] + xall, [("xm", slot, k)])
                    return [("xm", slot, k) for k in range(16)]

                def proj(lhs_of_k, M, rd_w, rd_x, slot, out_ps):
                    for k in range(16):
                        S.mm(out_ps, lhs_of_k(k), xm[slot][:, k, :], k == 0, k == 15, rd_w + rd_x, [("pjx", id(out_ps))])

                rdx = mix(3, 0)
                for k in range(16):
                    S.mm(plo[0:96, 0:TW], w1[:, k, :], xm[0][:, k, :], k == 0, k == 15, ["w1"] + rdx, ["plo"])
                S.act(hlo["w"][0:96, :], plo[0:96, 0:TW], AF.Tanh, ["plo"], ["hw"])
                rdx = mix(4, 1)
                for k in range(16):
                    S.mm(plo[0:96, 0:TW], a1[:, k, :], xm[1][:, k, :], k == 0, k == 15, ["a1"] + rdx, ["plo"])
                S.cp(ACT, hlo["a"][0:96, :], plo[0:96, 0:TW], ["plo"], ["ha"])
                rdx = mix(5, 0)
                for mt in range(2):
                    for k in range(16):
                        S.mm(plo[:, 0:TW], g1[:, k, mt * 128:(mt + 1) * 128], xm[0][:, k, :], k == 0, k == 15,
                             ["g1"] + rdx, ["plo"])
                    S.act(hg[:, mt, :], plo[:, 0:TW], AF.Sigmoid, ["plo"], [("hg", mt)])
                rd_r = mix(0, 1)
                rd_k = mix(1, 0)
                for ct in range(NCT):
                    i = nu % 2
                    nu += 1
                    cs = slice(ct * 128, (ct + 1) * 128)
                    R = lambda nm: (nm, i)
                    S.dma(POOL, wst["r"][i][:], dr["wrkv"][0, ct], [], [("Wr", i)])
                    S.dma(POOL, wst["k"][i][:], dr["wrkv"][1, ct], [], [("Wk", i)])
                    pi = npj % 3; npj += 1
                    for k in range(16):
                        S.mm(pj[pi][:, 0:TW], wst["r"][i][:, k, :], xm[1][:, k, :], k == 0, k == 15, [("Wr", i)] + rd_r,
                             [("pj", pi)])
                    S.cp(ACT, tr_[i][:], pj[pi][:, 0:TW], [("pj", pi)], [R("tr")])
                    pi = npj % 3; npj += 1
                    for k in range(16):
                        S.mm(pj[pi][:, 0:TW], wst["k"][i][:, k, :], xm[0][:, k, :], k == 0, k == 15, [("Wk", i)] + rd_k,
                             [("pj", pi)])
                    S.cp(ACT, tk_[i][:], pj[pi][:, 0:TW], [("pj", pi)], [R("tk")])
                    pi = npj % 3; npj += 1
                    S.mm(pj[pi][:, 0:TW], w2[:, cs], hlo["w"][0:96, :], True, True, ["w2", "hw"], [("pj", pi)])
                    S.act(tld[i][:], pj[pi][:, 0:TW], AF.Sigmoid, [("pj", pi), "cv"], [R("tld")], bias=cv["w0"][:, ct:ct + 1],
                          scale=1.0)
                    S.ts(DVE, tld[i][:], tld[i][:], -EW, None, ALU.mult, ALU.bypass, [R("tld")], [R("tld")])
                    pi = npj % 3; npj += 1
                    S.mm(pj[pi][:, 0:TW], a2[:, cs], hlo["a"][0:96, :], True, True, ["a2", "ha"], [("pj", pi)])
                    S.act(ta_[i][:], pj[pi][:, 0:TW], AF.Sigmoid, [("pj", pi), "cv"], [R("ta")], bias=cv["a0"][:, ct:ct + 1],
                          scale=1.0)
                    pi = npj % 3; npj += 1
                    for mt in range(2):
                        S.mm(pj[pi][:, 0:TW], g2[:, mt, cs], hg[:, mt, :], mt == 0, mt == 1, ["g2", ("hg", mt)],
                             [("pj", pi)])
                    S.cp(ACT, ob["g"][i][:], pj[pi][:, 0:TW], [("pj", pi)], [R("og")])
                    S.dma(SP, dr["gT"][:, ct, t0:t0 + TW], ob["g"][i][:], [R("og")], [("gT", ct, tt)], key=R("og_st"))
                    S.ts(DVE, tkk[i][:], tk_[i][:], cv["kk"][:, ct:ct + 1], None, ALU.mult, ALU.bypass, [R("tk"), "cv"],
                         [R("tkk")])
                    S.tt(DVE, tt1[i][:], tkk[i][:], tkk[i][:], ALU.mult, [R("tkk")], [R("tt1")])
                    pi = npj % 3; npj += 1
                    S.mm(pj[pi][:, 0:TW], bones[:], tt1[i][:], True, True, ["bones", R("tt1")], [("pj", pi)])
                    S.act(tt1[i][:], pj[pi][:, 0:TW], AF.Sqrt, [("pj", pi), "tiny"], [R("tt1")], bias=tiny[:], scale=1.0)
                    S.add(DVE, lambda e, i=i: e.reciprocal(tt1[i][:], tt1[i][:]), [R("tt1")], [R("tt1")])
                    S.tt(DVE, tkk[i][:], tkk[i][:], tt1[i][:], ALU.mult, [R("tkk"), R("tt1")], [R("tkk")])
                    S.tt(DVE, tb_[i][:], tkk[i][:], ta_[i][:], ALU.mult, [R("tkk"), R("ta")], [R("tb")])
                    S.ts(DVE, tt2[i][:], ta_[i][:], -1.0, cv["ka"][:, ct:ct + 1], ALU.add, ALU.mult, [R("ta"), "cv"],
                         [R("tt2")])
                    S.add(DVE, lambda e, i=i: e.scalar_tensor_tensor(tkm[i][:], tt2[i][:], 1.0, tk_[i][:], ALU.add,
                                                                     ALU.mult), [R("tt2"), R("tk")], [R("tkm")])
                    S.add(DVE, lambda e, i=i: e.tensor_tensor_scan(tlc[i][:], smask[:], tld[i][:], 0.0, ALU.mult,
                                                                   ALU.add), ["smask", R("tld")], [R("tlc")])
                    lc3 = tlc[i][:].rearrange("p (c t) -> p c t", t=64)
                    S.act(tt1[i][:], tlc[i][:], AF.Exp, [R("tlc")], [R("tt1")])
                    S.tt(DVE, oar[i][:, :, 64:128], tr_[i][:].rearrange("p (c t) -> p c t", t=64),
                         tt1[i][:].rearrange("p (c t) -> p c t", t=64), ALU.mult, [R("tr"), R("tt1")], [R("oarR")])
                    S.add(DVE, lambda e, i=i, lc3=lc3: e.tensor_copy(wend[i][:], lc3[:, :, 63]), [R("tt1"), R("tlc")], [R("wend")])
                    S.tt(DVE, tt2[i][:], tlc[i][:], tld[i][:], ALU.subtract, [R("tlc"), R("tld")], [R("tt2")])
                    S.act(tt2[i][:], tt2[i][:], AF.Exp, [R("tt2")], [R("tt2")])
                    S.add(DVE, lambda e, i=i: e.scalar_tensor_tensor(oar[i][:, :, 0:64],
                                                                     tkk[i][:].rearrange("p (c t) -> p c t", t=64), -1.0,
                                                                     tt2[i][:].rearrange("p (c t) -> p c t", t=64),
                                                                     ALU.mult, ALU.mult), [R("tkk"), R("tt2")], [R("oarA")])
                    S.act(tt1[i][:], tlc[i][:], AF.Exp, [R("tlc"), R("oarR")], [R("tt1")], scale=-1.0)
                    S.tt(DVE, ob["B"][i][:], tb_[i][:], tt1[i][:], ALU.mult, [R("tb"), R("tt1")], [R("oB")])
                    S.tt(DVE, ob["K"][i][:], tkm[i][:], tt1[i][:], ALU.mult, [R("tkm"), R("tt1")], [R("oK")])
                    S.tt(DVE, tt2[i][:].rearrange("p (c t) -> p c t", t=64),
                         wend[i][:].unsqueeze(2).to_broadcast([128, CPT, 64]), lc3, ALU.subtract,
                         [R("wend"), R("tlc"), R("oarA")], [R("tt2")])
                    S.act(tt2[i][:], tt2[i][:], AF.Exp, [R("tt2")], [R("tt2")])
                    S.tt(DVE, ob["Bh"][i][:], tb_[i][:], tt2[i][:], ALU.mult, [R("tb"), R("tt2")], [R("oBh")])
                    S.tt(DVE, ob["Kh"][i][:], tkm[i][:], tt2[i][:], ALU.mult, [R("tkm"), R("tt2")], [R("oKh")])
                    S.act(wend[i][:], wend[i][:], AF.Exp, [R("wend"), R("tt2")], [R("wend")])
                    S.dma(SP, dr["ARt"][:, ct, tt * CPT:(tt + 1) * CPT, :], oar[i][:], [R("oarA"), R("oarR")],
                          [("ARt", ct, tt)], key=R("oar_st"))
                    for nm in ("B", "K", "Bh", "Kh"):
                        S.dma(SP, dr[nm + "t"][:, ct, t0:t0 + TW], ob[nm][i][:], [R("o" + nm)], [(nm + "t", ct, tt)],
                              key=R("o%s_st" % nm))
                    S.dma(SP, dr["wend"][:, ct, tt * CPT:(tt + 1) * CPT], wend[i][:], [R("wend")], [("wendd", ct, tt)],
                          key=R("wend_st"))
                    S.dma(SP, dr["r32"][:, ct, t0:t0 + TW], tr_[i][:], [R("tr")], [("r32", ct, tt)], key=R("r32_st"))
                    S.dma(SP, dr["k32"][:, ct, t0:t0 + TW], tkm[i][:], [R("tkm")], [("k32", ct, tt)], key=R("k32_st"))
                rd_v = mix(2, 1)
                for ct in range(NCT):
                    i = nu % 2
                    nu += 1
                    cs = slice(ct * 128, (ct + 1) * 128)
                    pi = npj % 3; npj += 1
                    S.dma(POOL, wst["v"][i][:], dr["wrkv"][2, ct], [], [("Wv", i)])
                    for k in range(16):
                        S.mm(pj[pi][:, 0:TW], wst["v"][i][:, k, :], xm[1][:, k, :], k == 0, k == 15, [("Wv", i)] + rd_v,
                             [("pj", pi)])
                    S.cp(ACT, tv_[i][:], pj[pi][:, 0:TW], [("pj", pi)], [("tv", i)])
                    S.cp(DVE, ob["v"][i][:], tv_[i][:], [("tv", i)], [("ov", i)])
                    S.dma(SP, dr["v32"][:, ct, t0:t0 + TW], tv_[i][:], [("tv", i)], [("v32", ct, tt)], key=("v32_st", i))
                    S.dma(SP, dr["vt"][:, ct, t0:t0 + TW], ob["v"][i][:], [("ov", i)], [("vt", ct, tt)],
                          key=("ov_st", i))
        S.barrier()
        if RW_STOP <= 0:
            return
        NG = NCHK // GC
        U = 2 * GC
        with ExitStack() as st:
            fm = {n: [C.sb(st, [64, 2, GC * 64], BF16, "fm%s%d" % (n, i)) for i in range(2)] for n in
                  ("B", "K", "Bh", "Kh", "v")}
            far = [C.sb(st, [64, 2, GC, 128], BF16, "far%d" % i) for i in range(2)]
            pA1 = C.ps(st, [64, U, 64], F32, "pA1")
            pA2 = C.ps(st, [64, U, 128], F32, "pA2")
            pB = C.ps(st, [64, U, 128], F32, "pB")
            pC = C.ps(st, [64, U, 64], F32, "pC")
            pT = C.ps(st, [64, GC, 4, 128], BF16, "pT")
            N_ = [C.sb(st, [64, U, 64], BF16, "N%d" % i) for i in range(2)]
            LY = [C.sb(st, [64, U, 192], BF16, "LY%d" % i) for i in range(2)]
            BM = C.sb(st, [64, U, 128], BF16, "BM")
            AKt = C.sb(st, [64, U, 64], BF16, "AKt")
            MRKt = C.sb(st, [64, U, 64], BF16, "MRKt")
            Vt = C.sb(st, [64, U, 64], BF16, "Vt")
            Kh = C.sb(st, [64, U, 64], BF16, "Kht")
            oGT = C.sb(st, [64, U, 64], BF16, "oGT")
            oRp = C.sb(st, [64, U, 64], BF16, "oRp")
            oY0 = C.sb(st, [64, U, 64], F32, "oY0")
            oH = C.sb(st, [64, U, 64], F32, "oH")
            bc = lambda m: m[:].unsqueeze(1).to_broadcast([64, U, 64])
            v4 = lambda t, lo, hi: t[:, :, lo:hi].rearrange("p (c h) w -> p c h w", h=2)
            ng = 0
            for ct in range(NCT):
                for g in range(NG):
                    i = ng % RW_NB
                    ng += 1
                    c0 = g * GC
                    tt = (c0 * 64) // TW
                    R = lambda nm: (nm, i)
                    for nm in ("B", "K", "Bh", "Kh", "v"):
                        S.dma(SP, fm[nm][i][:], dr[nm + "t"][:, ct, c0 * 64:(c0 + GC) * 64].rearrange(
                            "(h p) c -> p h c", h=2), [(nm + "t", ct, tt)], [R("fm" + nm)])
                    S.dma(SP, far[i][:], dr["ARt"][:, ct, c0:c0 + GC, :].rearrange("(h p) c w -> p h c w", h=2),
                          [("ARt", ct, tt)], [R("far")])
                    lvl = RW_A1 if g >= 1 else RW_A0
                    if lvl <= 1:
                        continue
                    ua = lambda t, cc, hp: t[:, hp, cc * 64:(cc + 1) * 64]
                    for cc in range(GC):
                        for hp in range(RW_HP):
                            u = cc * 2 + hp
                            ps = slice(hp * 64, (hp + 1) * 64)
                            S.mm(pA2[:, u, :], ua(fm["B"][i], cc, hp), far[i][:, hp, cc, :], True, True,
                                 [R("fmB"), R("far")], ["pA2"])
                            S.mm(pB[:, u, :], ua(fm["K"][i], cc, hp), far[i][:, hp, cc, :], True, True,
                                 [R("fmK"), R("far")], ["pB"])
                            S.mm(pC[:, u, :], far[i][:, hp, cc, 0:64], ua(fm["B"][i], cc, hp), True, True,
                                 [R("fmB"), R("far")], ["pC"])
                    for cc in range(GC if RW_SUB >= 2 else 0):
                        for hp in range(2):
                            hs = slice(hp * 64, (hp + 1) * 64)
                            idb = identb[0:64, 0:64]
                            S.tr(pT[:, cc, 0, hs], far[i][:, hp, cc, 0:64], idb, [R("far"), "identb"], ["pT"])
                            S.tr(pT[:, cc, 1, hs], ua(fm["Bh"][i], cc, hp), idb, [R("fmBh"), "identb"], ["pT"])
                            S.tr(pT[:, cc, 2, hs], ua(fm["Kh"][i], cc, hp), idb, [R("fmKh"), "identb"], ["pT"])
                            S.tr(pT[:, cc, 3, hs], ua(fm["v"][i], cc, hp), idb, [R("fmv"), "identb"], ["pT"])
                    tsrc = lambda k: pT[:, :, k, :].rearrange("p c (h w) -> p c h w", h=2)
                    if RW_SUB >= 3:
                        S.cp(ACT, v4(LY[0], 64, 128), tsrc(0), ["pT"], ["LY0"])
                        S.cp(ACT, v4(BM, 0, 64), tsrc(1), ["pT"], ["BMb"])
                        S.cp(ACT, v4(Kh, 0, 64), tsrc(2), ["pT"], ["Kht"])
                        S.cp(ACT, v4(Vt, 0, 64), tsrc(3), ["pT"], ["Vt"])
                    if RW_SUB <= 3:
                        continue
                    S.tt(DVE, N_[0][:], pA2[:, :, 0:64], bc(m_su), ALU.mult, ["pA2", "masks"], ["N0"])
                    S.tt(DVE, BM[:, :, 64:128], pA2[:, :, 64:128], bc(m_ui), ALU.mult, ["pA2", "masks"], ["BMm"])
                    S.tt(DVE, AKt[:], pB[:, :, 0:64], bc(m_su), ALU.mult, ["pB", "masks"], ["AKt"])
                    S.tt(DVE, MRKt[:], pB[:, :, 64:128], bc(m_ui), ALU.mult, ["pB", "masks"], ["MRKt"])
                    S.tt(DVE, LY[0][:, :, 0:64], pC[:], bc(m_sl), ALU.mult, ["pC", "masks"], ["LY0"])
                    for u in range(U):
                        S.mm(pC[:, u, :], AKt[:, u, :], Vt[:, u, :], True, True, ["AKt", "Vt"], ["pC"])
                    S.cp(DVE, LY[0][:, :, 128:192], pC[:], ["pC"], ["LY0"])
                    if lvl <= 2:
                        continue
                    for j in range(6):
                        a, b = j % 2, (j + 1) % 2
                        last = j == 5
                        for u in range(U):
                            S.mm(pA2[:, u, :], N_[a][:, u, :], LY[a][:, u, 64:192], True, True,
                                 ["N%d" % a, "LY%d" % a], ["pA2"])
                            if not last:
                                S.mm(pA1[:, u, :], N_[a][:, u, :], LY[a][:, u, 0:64], True, True,
                                     ["N%d" % a, "LY%d" % a], ["pA1"])
                                S.mm(pC[:, u, :], LY[a][:, u, 0:64], N_[a][:, u, :], True, True,
                                     ["N%d" % a, "LY%d" % a], ["pC"])
                        S.tt(DVE, LY[b][:, :, 64:192], pA2[:], LY[a][:, :, 64:192], ALU.add,
                             ["pA2", "LY%d" % a], ["LY%d" % b])
                        if not last:
                            S.cp(ACT, LY[b][:, :, 0:64], pA1[:], ["pA1"], ["LY%d" % b])
                            S.cp(ACT, N_[b][:], pC[:], ["pC"], ["N%d" % b])
                    Yf = LY[0]
                    if lvl <= 3:
                        continue
                    for u in range(U):
                        S.mm(pB[:, u, :], Yf[:, u, 64:128], BM[:, u, :], True, True, ["LY0", "BMb", "BMm"], ["pB"])
                        S.mm(pA1[:, u, :], BM[:, u, 64:128], Yf[:, u, 128:192], True, False, ["LY0", "BMm"], ["pA1"])
                        S.mm(pA1[:, u, :], MRKt[:, u, :], Vt[:, u, :], False, True, ["MRKt", "Vt"], ["pA1"])
                        S.mm(pC[:, u, :], BM[:, u, 0:64], Yf[:, u, 128:192], True, False, ["LY0", "BMb"], ["pC"])
                        S.mm(pC[:, u, :], Kh[:, u, :], Vt[:, u, :], False, True, ["Kht", "Vt"], ["pC"])
                    S.cp(DVE, oGT[:], pB[:, :, 0:64], ["pB"], ["oGT"])
                    for hp in range(2):
                        S.tt(DVE, oRp[:].rearrange("p (c h) w -> p c h w", h=2)[:, :, hp, :],
                             pB[:, :, 64:128].rearrange("p (c h) w -> p c h w", h=2)[:, :, hp, :],
                             far[i][:, hp, :, 64:128], ALU.add, ["pB", R("far")], ["oRp"])
                    S.cp(ACT, oY0[:], pA1[:], ["pA1"], ["oY0"])
                    S.cp(DVE, oH[:], pC[:], ["pC"], ["oH"])
                    if lvl <= 4:
                        continue
                    osl = lambda d: d[c0:c0 + GC, :, 2 * ct:2 * ct + 2, :].rearrange("c p h w -> p c h w")
                    S.dma(SP, osl(dr["GT"]), oGT[:].rearrange("p (c h) w -> p c h w", h=2), ["oGT"], [("GT", ct, g)],
                          key="oGT_st")
                    S.dma(SP, osl(dr["RpT"]), oRp[:].rearrange("p (c h) w -> p c h w", h=2), ["oRp"], [("RpT", ct, g)],
                          key="oRp_st")
                    S.dma(SP, osl(dr["Y0p"]), oY0[:].rearrange("p (c h) w -> p c h w", h=2), ["oY0"], [("Y0p", ct, g)],
                          key="oY0_st")
                    S.dma(SP, osl(dr["Hm"]), oH[:].rearrange("p (c h) w -> p c h w", h=2), ["oH"], [("Hm", ct, g)],
                          key="oH_st")
        S.barrier()
        if RW_STOP <= 1:
            return
        with ExitStack() as st:
            wendS = C.sb(st, [64, NH, NCHK], F32, "wendS")
            for ct in range(NCT):
                for hp in range(2):
                    S.dma(SP, wendS[:, 2 * ct + hp, :], dr["wend"][hp * 64:(hp + 1) * 64, ct, :],
                          [("wendd", ct, tt) for tt in range(NTT)], [("wendSr", ct, hp)], key="ld_wendS")
            S.join([("wendSr", ct, hp) for ct in range(NCT) for hp in range(2)] + ["wendS"])
            P32 = C.sb(st, [64, NH, 64], F32, "P32")
            Pb = C.sb(st, [64, NH, 64], BF16, "Pb")
            S.add(POOL, lambda e: e.memset(P32[:], 0.0), [], ["P32"])
            S.add(POOL, lambda e: e.memset(Pb[:], 0.0), [], ["Pb"])
            gt_ = [C.sb(st, [64, NH, 64], BF16, "gt%d" % i) for i in range(2)]
            rp_ = [C.sb(st, [64, NH, 64], BF16, "rp%d" % i) for i in range(2)]
            y0_ = [C.sb(st, [64, NH, 64], F32, "y0%d" % i) for i in range(2)]
            hm_ = [C.sb(st, [64, NH, 64], F32, "hm%d" % i) for i in range(2)]
            yo_ = [C.sb(st, [64, NH * 64], F32, "yo%d" % i) for i in range(2)]
            pY = C.ps(st, [64, NH, 64], F32, "pY")
            pP = C.ps(st, [64, NH, 64], F32, "pP")
            pyt = [C.ps(st, [128, 512], F32) for _ in range(2)]
            yT = [C.sb(st, [128, NCT, 64], F32, "yTc%d" % i) for i in range(2)]
            allA = lambda nm, c: [(nm, ct, c // GC) for ct in range(NCT)]
            for c in range(NCHK):
                i = c % 2
                R = lambda nm: (nm, i)
                S.dma(SP, gt_[i][:], dr["GT"][c], allA("GT", c), [R("gt")])
                S.dma(SP, rp_[i][:], dr["RpT"][c], allA("RpT", c), [R("rp")])
                S.dma(SP, y0_[i][:], dr["Y0p"][c], allA("Y0p", c), [R("y0")])
                S.dma(SP, hm_[i][:], dr["Hm"][c], allA("Hm", c), [R("hm")])
                for h in range(NH):
                    S.mm(pY[:, h, :], rp_[i][:, h, :], Pb[:, h, :], True, True, [R("rp"), "Pb"], ["pY"])
                for h in range(NH):
                    S.mm(pP[:, h, :], gt_[i][:, h, :], Pb[:, h, :], True, True, [R("gt"), "Pb"], ["pP"])
                S.tt(DVE, yo_[i][:].rearrange("p (h w) -> p h w", w=64), pY[:], y0_[i][:], ALU.add, ["pY", R("y0")],
                     [R("yo")])
                S.tt(DVE, P32[:], P32[:], wendS[:, :, c:c + 1].to_broadcast([64, NH, 64]), ALU.mult,
                     ["P32", "wendS"], ["P32"])
                S.tt(DVE, P32[:], P32[:], pP[:], ALU.add, ["P32", "pP"], ["P32"])
                S.tt(DVE, P32[:], P32[:], hm_[i][:], ALU.add, ["P32", R("hm")], ["P32"])
                S.cp(ACT, Pb[:], P32[:], ["P32"], ["Pb"])
                for ct in range(NCT):
                    S.tr(pyt[i][:, ct * 64:(ct + 1) * 64], yo_[i][:, ct * 128:(ct + 1) * 128], identf[0:64, 0:64],
                         [R("yo"), "identf"], [("pyt", i)])
                S.cp(ACT, yT[i][:].rearrange("p c t -> p (c t)"), pyt[i][:, 0:NCT * 64], [("pyt", i)], [R("yTc")])
                S.dma(SP, dr["y32"][:, :, c * 64:(c + 1) * 64], yT[i][:], [R("yTc")], [("y32", c)], key=R("yTc_st"))
        S.barrier()
        if RW_STOP <= 2:
            return
        with ExitStack() as st:
            F = lambda nm: [C.sb(st, [128, TW], F32, "%s%d" % (nm, i)) for i in range(2)]
            c_y, c_r, c_k, c_v, c_m, c_q, c_s = F("c_y"), F("cr"), F("ck"), F("cv"), F("cm"), F("cq"), F("cs")
            c_g = [C.sb(st, [128, TW], BF16, "cg%d" % i) for i in range(2)]
            c_o = [C.sb(st, [128, TW], BF16, "co%d" % i) for i in range(2)]
            pm = [C.ps(st, [128, 512], F32) for _ in range(2)]
            pq = [C.ps(st, [128, 512], F32) for _ in range(2)]
            pb_ = [C.ps(st, [128, 512], F32) for _ in range(2)]
            n = 0
            for ct in range(NCT):
                for tt in range(NTT):
                    i = n % 2
                    n += 1
                    t0 = tt * TW
                    R = lambda nm: (nm, i)
                    S.dma(SP, c_y[i][:], dr["y32"][:, ct, t0:t0 + TW], [("y32", c) for c in range(tt * CPT, (tt + 1) * CPT)],
                          [R("c_y")])
                    S.dma(SP, c_r[i][:], dr["r32"][:, ct, t0:t0 + TW], [("r32", ct, tt)], [R("cr")])
                    S.dma(SP, c_k[i][:], dr["k32"][:, ct, t0:t0 + TW], [("k32", ct, tt)], [R("ck")])
                    S.dma(SP, c_v[i][:], dr["v32"][:, ct, t0:t0 + TW], [("v32", ct, tt)], [R("cv")])
                    S.dma(SP, c_g[i][:], dr["gT"][:, ct, t0:t0 + TW], [("gT", ct, tt)], [R("cg")])
                    S.mm(pm[i][:, 0:TW], bones[:], c_y[i][:], True, True, ["bones", R("c_y")], [("pm", i)])
                    S.tt(DVE, c_q[i][:], c_y[i][:], c_y[i][:], ALU.mult, [R("c_y")], [R("cq")])
                    S.mm(pq[i][:, 0:TW], bones[:], c_q[i][:], True, True, ["bones", R("cq")], [("pq", i)])
                    S.ts(DVE, c_m[i][:], pm[i][:, 0:TW], 1.0 / 64, None, ALU.mult, ALU.bypass, [("pm", i)], [R("cm")])
                    S.tt(DVE, c_s[i][:], c_m[i][:], c_m[i][:], ALU.mult, [R("cm")], [R("cs")])
                    S.add(DVE, lambda e, i=i: e.scalar_tensor_tensor(c_s[i][:], pq[i][:, 0:TW], 1.0 / 64, c_s[i][:],
                                                                     ALU.mult, ALU.subtract), [("pq", i), R("cs")],
                          [R("cs")])
                    S.act(c_s[i][:], c_s[i][:], AF.Sqrt, [R("cs"), "epsg"], [R("cs")], bias=epsg[:], scale=1.0)
                    S.add(DVE, lambda e, i=i: e.reciprocal(c_s[i][:], c_s[i][:]), [R("cs")], [R("cs")])
                    S.tt(DVE, c_y[i][:], c_y[i][:], c_m[i][:], ALU.subtract, [R("c_y"), R("cm")], [R("c_y")])
                    S.tt(DVE, c_y[i][:], c_y[i][:], c_s[i][:], ALU.mult, [R("c_y"), R("cs")], [R("c_y")])
                    S.ts(DVE, c_y[i][:], c_y[i][:], cv["gng"][:, ct:ct + 1], cv["gnb"][:, ct:ct + 1], ALU.mult, ALU.add,
                         [R("c_y"), "cv"], [R("c_y")])
                    S.add(DVE, lambda e, i=i, ct=ct: e.scalar_tensor_tensor(c_q[i][:], c_r[i][:], cv["rk"][:, ct:ct + 1],
                                                                            c_k[i][:], ALU.mult, ALU.mult),
                          [R("cr"), R("ck"), "cv", ("pq", i)], [R("cq")])
                    S.mm(pb_[i][:, 0:TW], bones[:], c_q[i][:], True, True, ["bones", R("cq")], [("pb", i)])
                    S.tt(DVE, c_q[i][:], pb_[i][:, 0:TW], c_v[i][:], ALU.mult, [("pb", i), R("cv")], [R("cq")])
                    S.tt(DVE, c_y[i][:], c_y[i][:], c_q[i][:], ALU.add, [R("c_y"), R("cq")], [R("c_y")])
                    S.tt(DVE, c_o[i][:], c_y[i][:], c_g[i][:], ALU.mult, [R("c_y"), R("cg")], [R("co")])
                    S.dma(SP, dr["ygT"][:, ct, t0:t0 + TW], c_o[i][:], [R("co")], [("ygT", ct, tt)], key=R("co_st"))


def front0_drams(nc, NBA, SCRK="ExternalOutput", YTK="ExternalOutput"):
    NBO = NBA // 2; TA = NBA * 128; NT = NBO * 128
    dt = lambda name, shape, d, kind="ExternalInput": nc.dram_tensor(name, shape, d, kind=kind).ap()
    return dict(
        xall=dt("xall", [TA, 2048], F32), wu=dt("wu", [8, 128, 16, 128], F32), wq=dt("wq", [8, 128, 16, 128], F32),
        wk=dt("wk", [8, 128, 16, 128], F32), wvs=dt("wvs", [2, 128, 16, 512], F32), wv=dt("wv", [2, 128, 16, 512], F32),
        wf=dt("wf", [128, 16, 8], F32), bau=dt("bau", [128, 8], F32), bav=dt("bav", [1024], F32),
        gv=dt("gv", [1024], F32), bv=dt("bv", [1024], F32), wsT=dt("wsT", [128, 8, 128], F32),
        bs=dt("bs", [1024], F32), bf=dt("bf", [8], F32), hf=dt("hf", [128, 2], F32),
        maskT=dt("maskT", [NBO, 128, NBA, 128], BF16),
        uTd=dt("uTd", [128, 8, NT], BF16, SCRK), qTd=dt("qTd", [128, 8, NT], BF16, SCRK),
        vsd=dt("vsd", [NT, 1024], BF16, SCRK), kT=dt("kT", [128, 8, TA], BF16, SCRK),
        V=dt("V", [TA, 1024], BF16, SCRK), yT=dt("yT", [128, 16, NT], BF16, YTK),
    )


def front0_host(x_true, h, w_in, b_a, w_s, b_s, g_v, b_v, b_f, NBA):
    NBO = NBA // 2; TA = NBA * 128; NT = NBO * 128
    xall = np.concatenate([x_true[h * NT:(h + 1) * NT], x_true[(1 - h) * NT:(2 - h) * NT]], 0)
    r = lambda a: np.ascontiguousarray(a.astype(np.float32))
    fm = lambda W: r(W.reshape(16, 128, -1, 128).transpose(2, 1, 0, 3))
    tm = lambda W: r(W.reshape(16, 128, -1, 512).transpose(2, 1, 0, 3))
    m = np.arange(TA); tp = (m + h * NT) % TA
    qtrue = tp[:NT].reshape(NBO, 1, 1, 128)
    ktrue = tp.reshape(NBA, 128).T.reshape(1, 128, NBA, 1)
    maskT = np.where(ktrue <= qtrue, 0.0, NEG).astype(ml_dtypes.bfloat16)
    return dict(
        xall=r(xall), wu=fm(w_in[:, 0:1024]), wq=fm(w_in[:, 2048:3072]), wk=fm(w_in[:, 3072:4096]),
        wvs=tm(w_in[:, 1024:2048]), wv=tm(w_in[:, 4096:5120]), wf=r(w_in[:, 5120:5128].reshape(16, 128, 8).transpose(1, 0, 2)),
        bau=r(b_a[:1024].reshape(8, 128).T), bav=r(b_a[1024:]), gv=r(g_v), bv=r(b_v),
        wsT=r(w_s.transpose(2, 0, 1)), bs=r(b_s.reshape(-1)), bf=r(b_f),
        hf=r(np.tile(np.array([[h, 1 - h]], np.float32), (128, 1))), maskT=np.ascontiguousarray(maskT))


def rwkv_drams(nc, T, NCT):
    NCHK = T // 64; NH = 2 * NCT
    dt = lambda name, shape, d, kind="ExternalInput": nc.dram_tensor(name, shape, d, kind=kind).ap()
    O = os.environ.get("SCR", "Internal")
    d = dict(
        x=dt("x", [T, 2048], F32), wrkv=dt("wrkv", [3, NCT, 128, 16, 128], F32), w1=dt("w1", [128, 16, 96], F32),
        a1=dt("a1", [128, 16, 96], F32), g1=dt("g1", [128, 16, 256], F32), w2=dt("w2", [96, NCT * 128], F32),
        a2=dt("a2", [96, NCT * 128], F32), g2=dt("g2", [128, 2, NCT * 128], F32), mu=dt("mu", [128, 6, 16], F32),
        ARt=dt("ARt", [128, NCT, NCHK, 128], BF16, O), wend=dt("wend", [128, NCT, NCHK], F32, O),
        gT=dt("gT", [128, NCT, T], BF16, O), GT=dt("GT", [NCHK, 64, NH, 64], BF16, O), RpT=dt("RpT", [NCHK, 64, NH, 64], BF16, O),
        Y0p=dt("Y0p", [NCHK, 64, NH, 64], F32, O), Hm=dt("Hm", [NCHK, 64, NH, 64], F32, O),
        ygT=dt("ygT", [128, NCT, T], BF16, "ExternalOutput"),
    )
    for nm in ("w0", "a0", "kk", "ka", "rk", "gng", "gnb"):
        d[nm] = dt(nm, [128, NCT], F32)
    for nm in ("Bt", "Kt", "Bht", "Kht", "vt"):
        d[nm] = dt(nm, [128, NCT, T], BF16, O)
    for nm in ("r32", "k32", "v32", "y32"):
        d[nm] = dt(nm, [128, NCT, T], F32, O)
    return d


def rwkv_host(x, p, ct0, NCT):
    r = lambda a: np.ascontiguousarray(np.asarray(a, np.float32))
    c0, c1 = ct0 * 128, (ct0 + NCT) * 128
    fm = lambda W: r(W.reshape(16, 128, -1).transpose(1, 0, 2))
    wrkv = np.stack([p["w_rkv"][i][:, c0:c1].reshape(16, 128, NCT, 128).transpose(2, 1, 0, 3) for i in range(3)], 0)
    col = lambda v: r(np.asarray(v).reshape(-1)[c0:c1].reshape(NCT, 128).T)
    return dict(
        x=r(x), wrkv=r(wrkv), w1=fm(p["w1"]), a1=fm(p["a1"]), g1=fm(p["g1"]), w2=r(p["w2"][:, c0:c1]), a2=r(p["a2"][:, c0:c1]),
        g2=r(p["g2"][:, c0:c1].reshape(2, 128, NCT * 128).transpose(1, 0, 2)), mu=r(p["mu"].reshape(6, 16, 128).transpose(2, 0, 1)),
        w0=col(p["w0"]), a0=col(p["a0"]), kk=col(p["k_k"]), ka=col(p["k_a"]), rk=col(p["r_k"]), gng=col(p["gn_g"]), gnb=col(p["gn_b"]))


def _tail_drams(nc, NT, with_y):
    dt = lambda name, shape, d, kind="ExternalInput": nc.dram_tensor(name, shape, d, kind=kind).ap()
    d = dict(
        wo=dt("wo", [128, 16, 2048], F32), lng=dt("lng", [2, 2048], F32), lnb=dt("lnb", [2, 2048], F32),
        wr=dt("wr", [128, 16, 16], F32), br=dt("br", [16], F32), wgu=dt("wgu", [16, 8, 128, 16, 2, 128], F32),
        wd=dt("wd", [16, 4, 128, 8, 512], F32), x1s=dt("x1s", [NT, 2048], F32, "Internal"),
        x1T=dt("x1T", [128, 16, NT], BF16, "Internal"), out=dt("out", [NT, 2048], F32, "ExternalOutput"))
    if with_y:
        d["yT"] = dt("yT", [128, 16, NT], BF16)
        d["xres"] = dt("xres", [NT, 2048], F32)
    return d


def _build_L1():
    nc = bass.Bass("TRN2", target_bir_lowering=False)
    NBA, NT = 32, 2048
    dr = front0_drams(nc, NBA, "Internal", "Internal")
    dr.update(_tail_drams(nc, NT, False))
    dr["xres"] = dr["xall"][0:NT, :]
    S = Sched(nc)
    C = Ctx(nc)
    with ExitStack() as st:
        identf, identb = make_ident(S, C, st)
        emit_front0(S, C, nc, NBA, dr, identf, identb)
        emit_tail(S, C, nc, NT, 1024, dr, 0, identf, identb)
        S.finalize([("out", b) for b in range(NT // 128)])
    return nc


def _build_L2():
    nc = bass.Bass("TRN2", target_bir_lowering=False)
    dr = rwkv_drams(nc, 4096, 8)
    S = Sched(nc)
    C = Ctx(nc)
    with ExitStack() as st:
        identf, identb = make_ident(S, C, st)
        emit_rwkv(S, C, nc, 4096, 8, dr, identf, identb)
        S.finalize([("ygT", ct, tt) for ct in range(8) for tt in range(8)])
    return nc


def _build_L3():
    nc = bass.Bass("TRN2", target_bir_lowering=False)
    NT = 2048
    dr = _tail_drams(nc, NT, True)
    S = Sched(nc)
    C = Ctx(nc)
    with ExitStack() as st:
        identf, identb = make_ident(S, C, st)
        emit_tail(S, C, nc, NT, 1024, dr, 1, identf, identb)
        S.finalize([("out", b) for b in range(NT // 128)])
    return nc


def _tail_host(W, lng, lnb, w_router, b_router, w_gu, w_down):
    r = lambda a: np.ascontiguousarray(np.asarray(a, np.float32))
    return dict(
        wo=r(np.asarray(W).reshape(16, 128, 2048).transpose(1, 0, 2)), lng=r(lng), lnb=r(lnb),
        wr=r(np.asarray(w_router).reshape(16, 128, 16).transpose(1, 0, 2)), br=r(b_router),
        wgu=r(np.asarray(w_gu).reshape(16, 16, 128, 2, 8, 128).transpose(0, 4, 2, 1, 3, 5)),
        wd=r(np.asarray(w_down).reshape(16, 8, 128, 4, 512).transpose(0, 3, 2, 1, 4)))


def kernel(x, ev_w_in, ev_b_a, ev_w_s, ev_b_s, ev_g_v, ev_b_v, ev_b_f, ev_w_out,
           rw_mu, rw_w_rkv, rw_w0, rw_w1, rw_w2, rw_a0, rw_a1, rw_a2, rw_g1, rw_g2,
           rw_k_k, rw_k_a, rw_r_k, rw_gn_g, rw_gn_b, rw_w_o,
           ln_g, ln_b, w_router, b_router, w_gu, w_down):
    n = 8
    NT = 2048
    x = np.asarray(x, np.float32)
    A = lambda a: np.asarray(a, np.float32)
    t0 = _tail_host(ev_w_out[0], ln_g[0], ln_b[0], w_router, b_router, w_gu[0], w_down[0])
    in_maps = []
    shared = None
    for c in range(n):
        b, h = c // 2, c % 2
        f = front0_host(x[b], h, A(ev_w_in[0]), A(ev_b_a[0]), A(ev_w_s[0]), A(ev_b_s[0]), A(ev_g_v[0]), A(ev_b_v[0]),
                        A(ev_b_f[0]), 32)
        if shared is None:
            shared = {k: v for k, v in f.items() if k not in ("xall", "hf", "maskT")}
        else:
            for k in shared:
                f[k] = shared[k]
        f.update(t0)
        in_maps.append(f)
    res = run_bass_kernel_spmd(_build_L1(), in_maps, core_ids=list(range(n)))
    x1 = np.stack([np.asarray(res.results[c]["out"], np.float32) for c in range(n)], 0).reshape(4, 4096, 2048)
    del res, in_maps, shared, t0
    p = dict(mu=A(rw_mu[0]), w_rkv=A(rw_w_rkv[0]), w0=A(rw_w0[0]), w1=A(rw_w1[0]), w2=A(rw_w2[0]), a0=A(rw_a0[0]),
             a1=A(rw_a1[0]), a2=A(rw_a2[0]), g1=A(rw_g1[0]), g2=A(rw_g2[0]), k_k=A(rw_k_k[0]), k_a=A(rw_k_a[0]),
             r_k=A(rw_r_k[0]), gn_g=A(rw_gn_g[0]), gn_b=A(rw_gn_b[0]))
    in_maps = [rwkv_host(x1[c // 2], p, (c % 2) * 8, 8) for c in range(n)]
    res = run_bass_kernel_spmd(_build_L2(), in_maps, core_ids=list(range(n)))
    yg = [np.asarray(res.results[c]["ygT"]) for c in range(n)]
    del res, in_maps
    t1 = _tail_host(rw_w_o[0], ln_g[1], ln_b[1], w_router, b_router, w_gu[1], w_down[1])
    in_maps = []
    for c in range(n):
        b, h = c // 2, c % 2
        yT = np.ascontiguousarray(np.concatenate([yg[2 * b][:, :, h * NT:(h + 1) * NT],
                                                  yg[2 * b + 1][:, :, h * NT:(h + 1) * NT]], axis=1))
        m = dict(t1)
        m["yT"] = yT
        m["xres"] = np.ascontiguousarray(x1[b, h * NT:(h + 1) * NT])
        in_maps.append(m)
    res = run_bass_kernel_spmd(_build_L3(), in_maps, core_ids=list(range(n)))
    out = np.stack([np.asarray(res.results[c]["out"], np.float32) for c in range(n)], 0).reshape(4, 4096, 2048)
    return out.astype(np.float32)
```

```python
import numpy as np
import concourse.bass as bass
import concourse.mybir as mybir
from concourse.bass_utils import run_bass_kernel_spmd

F32 = mybir.dt.float32
BF16 = mybir.dt.bfloat16
I32 = mybir.dt.int32
AF = mybir.ActivationFunctionType
ALU = mybir.AluOpType
AX = mybir.AxisListType

PE, ACT, DVE, POOL, SP = "tensor", "scalar", "vector", "gpsimd", "sync"
ENGS = [PE, ACT, DVE, POOL, SP]


class Op:
    __slots__ = ("eng", "emit", "deps", "is_dma", "key", "sig", "idx")

    def __init__(self, eng, emit, is_dma, key):
        self.eng = eng
        self.emit = emit
        self.deps = []
        self.is_dma = is_dma
        self.key = key
        self.sig = None
        self.idx = None


class Sched:
    def __init__(self, nc):
        self.nc = nc
        self.ops = []
        self.last_w = {}
        self.readers = {}
        self.last_by_key = {}
        self.barrier_deps = []

    def join(self, resources):
        last = None
        for op in reversed(self.ops):
            if op.is_dma:
                last = op
                break
        for r in resources:
            self.last_w[r] = last

    def barrier(self):
        self.barrier_deps = list(self.last_by_key.values())

    def add(self, eng, emit, reads=(), writes=(), dma=False, key=None):
        if dma:
            key = ("dma", key if key is not None else (tuple(writes)[0] if len(writes) else tuple(reads)[0]))
        op = Op(eng, emit, dma, key)
        deps = []
        for r in list(reads) + list(writes):
            w = self.last_w.get(r)
            if w is not None:
                deps.append(w)
        for w in writes:
            deps.extend(self.readers.get(w, ()))
        deps.extend(self.barrier_deps)
        seen = set()
        for d in deps:
            if id(d) in seen:
                continue
            seen.add(id(d))
            if d.eng == PE and eng == PE and not d.is_dma and not dma:
                continue
            op.deps.append(d)
        for w in writes:
            self.last_w[w] = op
            self.readers[w] = []
        for r in reads:
            lst = self.readers.setdefault(r, [])
            if not dma:
                lst[:] = [o for o in lst if o.is_dma or o.eng != eng]
            lst.append(op)
        self.ops.append(op)
        self.last_by_key[key if dma else ("eng", eng)] = op
        return op

    def mm(self, out, lhsT, rhs, start, stop, reads, writes, **kw):
        return self.add(PE, lambda e: e.matmul(out, lhsT, rhs, start=start, stop=stop, **kw), reads, writes)

    def tr(self, out, in_, ident, reads, writes):
        return self.add(PE, lambda e: e.transpose(out, in_, ident), reads, writes)

    def act(self, out, in_, func, reads, writes, **kw):
        return self.add(ACT, lambda e: e.activation(out, in_, func, **kw), reads, writes)

    def tt(self, eng, out, in0, in1, op, reads, writes):
        return self.add(eng, lambda e: e.tensor_tensor(out, in0, in1, op), reads, writes)

    def ts(self, eng, out, in0, s1, s2, op0, op1, reads, writes, **kw):
        return self.add(eng, lambda e: e.tensor_scalar(out, in0, s1, s2, op0, op1, **kw), reads, writes)

    def cp(self, eng, out, in_, reads, writes):
        if eng == ACT:
            return self.add(ACT, lambda e: e.copy(out, in_), reads, writes)
        return self.add(eng, lambda e: e.tensor_copy(out, in_), reads, writes)

    def dma(self, eng, out, in_, reads, writes, key=None, **kw):
        if eng == POOL and "max_dma_last_dim" not in kw:
            kw["max_dma_last_dim"] = 4096
        return self.add(eng, lambda e: e.dma_start(out=out, in_=in_, **kw), reads, writes, dma=True, key=key)

    def finalize(self, final_wait_keys=()):
        nc = self.nc
        for op in self.ops:
            if op.is_dma:
                op.sig = True
            for d in op.deps:
                d.sig = True
        final_ops = []
        for r in final_wait_keys:
            w = self.last_w.get(r)
            if w is not None:
                w.sig = True
                final_ops.append(w)
        sem_names = {}
        counts = {}
        for op in self.ops:
            if op.sig:
                k = op.key if op.is_dma else ("eng", op.eng)
                sem_names.setdefault(k, len(sem_names))
                counts[k] = counts.get(k, 0) + 1
                op.idx = counts[k]
        self.n_sems = len(sem_names)
        per_eng = {e: [] for e in ENGS}
        for op in self.ops:
            per_eng[op.eng].append(op)
        from contextlib import ExitStack
        with ExitStack() as st:
            sems = {}
            for k, i in sem_names.items():
                sems[k] = st.enter_context(nc.semaphore("s%d" % i))
            block = st.enter_context(nc.Block())

            def run(eng_name, e):
                waited = {}
                for op in per_eng[eng_name]:
                    need = {}
                    for d in op.deps:
                        k = d.key if d.is_dma else ("eng", d.eng)
                        v = d.idx * (16 if d.is_dma else 1)
                        if need.get(k, 0) < v:
                            need[k] = v
                    for k, v in need.items():
                        if waited.get(k, 0) >= v:
                            continue
                        e.wait_ge(sems[k], v)
                        waited[k] = v
                    ins = op.emit(e)
                    if op.sig:
                        k = op.key if op.is_dma else ("eng", op.eng)
                        ins.then_inc(sems[k], 16 if op.is_dma else 1)
                if eng_name == SP:
                    for w in final_ops:
                        k = w.key if w.is_dma else ("eng", w.eng)
                        v = w.idx * (16 if w.is_dma else 1)
                        e.wait_ge(sems[k], v)

            @block.tensor
            def _(e):
                run(PE, e)

            @block.scalar
            def _(e):
                run(ACT, e)

            @block.vector
            def _(e):
                run(DVE, e)

            @block.gpsimd
            def _(e):
                run(POOL, e)

            @block.sync
            def _(e):
                run(SP, e)

import ml_dtypes

from contextlib import ExitStack

D = 2048
NE = 16
DE = 1024
ALPHA = 4 ** 0.25
LN_EPS = 1e-5
NWGU = 2
NWD = 2
import os
DEBUG = int(os.environ.get('TAIL_DEBUG', '0'))
CUT = int(os.environ.get('TAIL_CUT', '9'))


class Ctx:
    def __init__(self, nc):
        self.nc = nc
        self.n = 0

    def sb(self, st, shape, dt, name=None):
        self.n += 1
        return st.enter_context(self.nc.sbuf_tensor("sb_%s_%d" % (name or "t", self.n), shape, dt))

    def ps(self, st, shape, dt, name=None):
        self.n += 1
        return st.enter_context(self.nc.psum_tensor("ps_%s_%d" % (name or "p", self.n), shape, dt))


def make_ident(S, C, st):
    identf = C.sb(st, [128, 128], F32, "identf")
    identb = C.sb(st, [128, 128], BF16, "identb")
    S.add(POOL, lambda e: e.memset(identf[:], 0.0), [], ["identf"])
    S.add(POOL, lambda e: e.affine_select(identf[:], identf[:], [[-1, 128]], ALU.not_equal, 1.0, base=0,
                                          channel_multiplier=1), ["identf"], ["identf"])
    S.cp(DVE, identb[:], identf[:], ["identf"], ["identb"])
    return identf, identb


def emit_ln(S, C, src, dst, gbc, bbc, tmp, reads, writes, eps, tag, ncol=2048, xn_eng=DVE, aff_eng=POOL):
    nch = ncol // 512
    st6, mv, sq, rstd = tmp["st6"], tmp["mv"], tmp["sq"], tmp["rstd"]
    r_st = tag + "_st"
    for c in range(nch):
        S.add(DVE, lambda e, c=c: e.bn_stats(st6[:, c, :], src[:, c * 512:(c + 1) * 512]), reads, [(r_st, c)])
    S.add(DVE, lambda e: e.bn_aggr(mv[:], st6[:, 0:nch, :].rearrange("p c s -> p (c s)")),
          [(r_st, c) for c in range(nch)], [tag + "_mv"])
    S.act(sq[:], mv[:, 1:2], AF.Sqrt, [tag + "_mv", "epsc"], [tag + "_sq"], bias=tmp["epsc"][:], scale=1.0)
    S.add(DVE, lambda e: e.reciprocal(rstd[:], sq[:]), [tag + "_sq"], [tag + "_rstd"])
    S.ts(xn_eng, src, src, mv[:, 0:1], rstd[:], ALU.subtract, ALU.mult, list(reads) + [tag + "_mv", tag + "_rstd"],
         list(reads))
    S.tt(aff_eng, src, src, gbc, ALU.mult, list(reads) + ["lnconst"], list(reads))
    S.tt(aff_eng, dst, src, bbc, ALU.add, list(reads) + ["lnconst"], writes)


def emit_tail(S, C, nc, NT, PASS, dr, lay, identf, identb):
    NB = NT // 128
    TTW = min(512, PASS)
    with ExitStack() as st0:
        gates = C.sb(st0, [128, NB, 16], F32, "gates")
        epsc = C.sb(st0, [128, 1], F32, "epsc")
        S.add(POOL, lambda e: e.memset(epsc[:], LN_EPS), [], ["epsc"])
        lng = C.sb(st0, [128, 2, 2048], F32, "lng")
        lnb = C.sb(st0, [128, 2, 2048], F32, "lnb")
        for i in range(2):
            S.dma(SP, lng[:, i, :], dr["lng"][i].partition_broadcast(128), [], ["lnconst"], key=("lnc", i))
            S.dma(SP, lnb[:, i, :], dr["lnb"][i].partition_broadcast(128), [], ["lnconst"], key=("lnc", 2 + i))
        lntmp = [dict(st6=C.sb(st0, [128, 4, 6], F32), mv=C.sb(st0, [128, 2], F32), sq=C.sb(st0, [128, 1], F32),
                      rstd=C.sb(st0, [128, 1], F32), epsc=epsc) for _ in range(2)]
        with ExitStack() as st:
            wo = C.sb(st, [128, 16, 2048], BF16, "wo")
            for h in range(4):
                S.dma(POOL, wo[:, 4 * h:4 * h + 4, :], dr["wo"][:, 4 * h:4 * h + 4, :], [], [("wo", h)])
            wr = C.sb(st, [128, 16, 16], F32, "wr")
            S.dma(SP, wr[:], dr["wr"], [], ["wr"])
            brb = C.sb(st, [128, 16], F32, "brb")
            S.dma(SP, brb[:], dr["br"].partition_broadcast(128), [], ["brb"])
            yTt = [C.sb(st, [128, 16, 128], BF16) for _ in range(2)]
            xr = [C.sb(st, [128, 2048], F32) for _ in range(2)]
            xT32 = [C.sb(st, [128, 16, 128], F32) for _ in range(2)]
            xT16 = [C.sb(st, [128, 16, 128], BF16) for _ in range(2)]
            pmix = [C.ps(st, [128, 2048], F32) for _ in range(1)]
            ptr = [C.ps(st, [128, 4, 128], F32) for _ in range(2)]
            plogf = C.ps(st, [128, 512], F32, "plog")
            plog = plogf[:, 0:16]
            rt = {k: C.sb(st, shp, F32, "rt_" + k) for k, shp in
                  dict(l=[128, 16], m=[128, 1], e=[128, 16], g1=[128, 4], mk=[128, 16], e2=[128, 16], g2=[128, 4],
                       gs=[128, 4], gm=[128, 1], gk=[128, 4], s1=[128, 16], rg=[128, 1]).items()}
            for b in range(NB):
                i = b % 2
                R = lambda n: (n, i)
                S.dma(SP, yTt[i][:], dr["yT"][:, :, b * 128:(b + 1) * 128], [], [R("yTt")])
                S.dma(SP, xr[i][:], dr["xres"][b * 128:(b + 1) * 128, :], [], [R("xr")])
                pm = pmix[0]
                for cb in range(4):
                    for k in range(16):
                        S.mm(pm[:, cb * 512:(cb + 1) * 512], yTt[i][:, k, :], wo[:, k, cb * 512:(cb + 1) * 512],
                             k == 0, k == 15, [R("yTt"), ("wo", k // 4)], [("pmix", cb)])
                for cb in range(4):
                    S.add(DVE, lambda e, cb=cb, i=i: e.scalar_tensor_tensor(
                        xr[i][:, cb * 512:(cb + 1) * 512], xr[i][:, cb * 512:(cb + 1) * 512], ALPHA,
                        pm[:, cb * 512:(cb + 1) * 512], ALU.mult, ALU.add), [R("xr"), ("pmix", cb)], [R("xr")])
                if CUT >= 2:
                    emit_ln(S, C, xr[i][:], xr[i][:], lng[:, 0, :], lnb[:, 0, :], lntmp[i], [R("xr")], [R("xr")], LN_EPS,
                            "ln1_%d" % i)
                S.dma(SP, dr["x1s"][b * 128:(b + 1) * 128, :], xr[i][:], [R("xr")], [("x1s", b)], key=R("xr_st"))
                if DEBUG == 1:
                    S.dma(SP, dr["out"][b * 128:(b + 1) * 128, :], xr[i][:], [R("xr")], [("out", b)], key=R("xr_st2"))
                if CUT < 3:
                    continue
                for q in range(4):
                    pt = ptr[q % 2]
                    for k in range(4 * q, 4 * q + 4):
                        S.tr(pt[:, k - 4 * q, :], xr[i][:, k * 128:(k + 1) * 128], identf[:], [R("xr"), "identf"],
                             [("ptr", q % 2)])
                    S.cp(ACT, xT32[i][:, 4 * q:4 * q + 4, :], pt[:], [("ptr", q % 2)], [R("xT32")])
                    S.cp(DVE, xT16[i][:, 4 * q:4 * q + 4, :], xT32[i][:, 4 * q:4 * q + 4, :], [R("xT32")], [R("xT16")])
                S.dma(SP, dr["x1T"][:, :, b * 128:(b + 1) * 128], xT16[i][:], [R("xT16")], [("x1T", b)], key=R("xT16_st"))
                if CUT < 4:
                    continue
                for k in range(16):
                    S.mm(plog, xT32[i][:, k, :], wr[:, k, :], k == 0, k == 15, [R("xT32"), "wr"], ["plog"])
                t = rt
                S.tt(DVE, t["l"][:], plog, brb[:], ALU.add, ["plog", "brb"], ["rt_l"])
                S.add(DVE, lambda e: e.tensor_reduce(t["m"][:], t["l"][:], AX.X, ALU.max), ["rt_l"], ["rt_m"])
                S.ts(DVE, t["m"][:], t["m"][:], -1.0, None, ALU.mult, ALU.bypass, ["rt_m"], ["rt_m"])
                S.act(t["e"][:], t["l"][:], AF.Exp, ["rt_l", "rt_m"], ["rt_e"], bias=t["m"][:], scale=1.0)
                e3 = t["e"][:].rearrange("p (g e) -> p g e", g=4)
                S.add(DVE, lambda e: e.tensor_reduce(t["g1"][:], e3, AX.X, ALU.max), ["rt_e"], ["rt_g1"])
                S.tt(DVE, t["mk"][:].rearrange("p (g e) -> p g e", g=4), e3,
                     t["g1"][:].unsqueeze(2).to_broadcast([128, 4, 4]), ALU.is_equal, ["rt_e", "rt_g1"], ["rt_mk"])
                S.add(DVE, lambda e: e.scalar_tensor_tensor(t["e2"][:], t["mk"][:], -1e30, t["e"][:], ALU.mult,
                                                            ALU.add), ["rt_mk", "rt_e"], ["rt_e2"])
                S.add(DVE, lambda e: e.tensor_reduce(t["g2"][:], t["e2"][:].rearrange("p (g e) -> p g e", g=4), AX.X,
                                                     ALU.max), ["rt_e2"], ["rt_g2"])
                S.tt(DVE, t["gs"][:], t["g1"][:], t["g2"][:], ALU.add, ["rt_g1", "rt_g2"], ["rt_gs"])
                S.add(DVE, lambda e: e.tensor_reduce(t["gm"][:], t["gs"][:], AX.X, ALU.max), ["rt_gs"], ["rt_gm"])
                S.ts(DVE, t["gk"][:], t["gs"][:], t["gm"][:], None, ALU.is_equal, ALU.bypass, ["rt_gs", "rt_gm"],
                     ["rt_gk"])
                S.tt(DVE, t["s1"][:].rearrange("p (g e) -> p g e", g=4), e3,
                     t["g2"][:].unsqueeze(2).to_broadcast([128, 4, 4]), ALU.is_ge, ["rt_e", "rt_g2"], ["rt_s1"])
                S.tt(DVE, t["s1"][:].rearrange("p (g e) -> p g e", g=4),
                     t["s1"][:].rearrange("p (g e) -> p g e", g=4),
                     t["gk"][:].unsqueeze(2).to_broadcast([128, 4, 4]), ALU.mult, ["rt_s1", "rt_gk"], ["rt_s1"])
                S.add(DVE, lambda e: e.reciprocal(t["rg"][:], t["gm"][:]), ["rt_gm"], ["rt_rg"])
                S.add(DVE, lambda e, b=b: e.scalar_tensor_tensor(gates[:, b, :], t["s1"][:], t["rg"][:], t["e"][:],
                                                                 ALU.mult, ALU.mult), ["rt_s1", "rt_rg", "rt_e"],
                      [("gates", b)])
        if DEBUG == 1:
            return
        S.barrier()
        NP = NT // PASS
        NBP = PASS // 128
        NTT = PASS // TTW
        with ExitStack() as st:
            xT = C.sb(st, [128, 16, PASS], BF16, "moe_xT")
            yacc = C.sb(st, [128, NBP, 2048], F32, "yacc")
            hb = [C.sb(st, [128, 8, PASS], BF16) for _ in range(2)]
            wgu = [C.sb(st, [128, 16, 2, 128], BF16) for _ in range(NWGU)]
            wd = [C.sb(st, [128, 8, 512], BF16) for _ in range(NWD)]
            sg = [C.sb(st, [128, TTW], F32) for _ in range(2)]
            pg = [C.ps(st, [128, 512], F32)[:, 0:TTW] for _ in range(2)]
            pu = [C.ps(st, [128, 512], F32)[:, 0:TTW] for _ in range(2)]
            py = [C.ps(st, [128, 512], F32) for _ in range(2)]
            xo = [C.sb(st, [128, 2048], F32) for _ in range(1)]
            ngu = 0
            nd = 0
            nps = 0
            npy = 0
            for p in range(NP):
                for k4 in range(4):
                    S.dma(SP, xT[:, 4 * k4:4 * k4 + 4, :], dr["x1T"][:, 4 * k4:4 * k4 + 4, p * PASS:(p + 1) * PASS],
                          [("x1T", b) for b in range(p * NBP, (p + 1) * NBP)], [("moe_xT", k4)])
                for ex in range(NE):
                    hi = ex % 2
                    for f in range(8):
                        wi = ngu % NWGU
                        ngu += 1
                        S.dma(POOL, wgu[wi][:], dr["wgu"][ex, f], [], [("wgu", wi)])
                        for tt in range(NTT):
                            pi = nps % 2
                            nps += 1
                            for k in range(16):
                                S.mm(pg[pi], wgu[wi][:, k, 0, :], xT[:, k, tt * TTW:(tt + 1) * TTW], k == 0, k == 15,
                                     [("wgu", wi), ("moe_xT", k // 4)], [("pg", pi)])
                            for k in range(16):
                                S.mm(pu[pi], wgu[wi][:, k, 1, :], xT[:, k, tt * TTW:(tt + 1) * TTW], k == 0, k == 15,
                                     [("wgu", wi), ("moe_xT", k // 4)], [("pu", pi)])
                            S.act(sg[pi][:], pg[pi], AF.Silu, [("pg", pi)], [("sg", pi)])
                            S.tt(DVE, hb[hi][:, f, tt * TTW:(tt + 1) * TTW], sg[pi][:], pu[pi], ALU.mult,
                                 [("sg", pi), ("pu", pi)], [("hb", hi, f)])
                    for cb in range(4):
                        di = nd % NWD
                        nd += 1
                        S.dma(POOL, wd[di][:], dr["wd"][ex, cb], [], [("wd", di)])
                        for bb in range(NBP):
                            b = p * NBP + bb
                            yi = npy % 2
                            npy += 1
                            for k in range(8):
                                S.mm(py[yi][:], hb[hi][:, k, bb * 128:(bb + 1) * 128], wd[di][:, k, :], k == 0, k == 7,
                                     [("hb", hi, k), ("wd", di)], [("py", yi)])
                            ysl = yacc[:, bb, cb * 512:(cb + 1) * 512]
                            if ex == 0:
                                S.ts(DVE, ysl, py[yi][:], gates[:, b, ex:ex + 1], None, ALU.mult, ALU.bypass,
                                     [("py", yi), ("gates", b)], [("yacc", bb, cb)])
                            else:
                                S.add(DVE, lambda e, ysl=ysl, yi=yi, b=b, ex=ex: e.scalar_tensor_tensor(
                                    ysl, py[yi][:], gates[:, b, ex:ex + 1], ysl, ALU.mult, ALU.add),
                                      [("py", yi), ("gates", b), ("yacc", bb, cb)], [("yacc", bb, cb)])
                for bb in range(NBP):
                    b = p * NBP + bb
                    i = 0
                    S.dma(SP, xo[i][:], dr["x1s"][b * 128:(b + 1) * 128, :], [("x1s", b)], [("xo", i)])
                    S.add(DVE, lambda e, i=i, bb=bb: e.scalar_tensor_tensor(
                        xo[i][:], xo[i][:], ALPHA, yacc[:, bb, :], ALU.mult, ALU.add),
                          [("xo", i)] + [("yacc", bb, cb) for cb in range(4)], [("xo", i)])
                    emit_ln(S, C, xo[i][:], xo[i][:], lng[:, 1, :], lnb[:, 1, :], lntmp[i], [("xo", i)], [("xo", i)],
                            LN_EPS, "ln2_%d" % i)
                    S.dma(SP, dr["out"][b * 128:(b + 1) * 128, :], xo[i][:], [("xo", i)], [("out", b)],
                          key=("xo_st", i))


SCALE = 128 ** -0.5
NEG = -30000.0


def emit_gelu(S, src_ps, bias_ap, bias_is_col, dst, tmp, rd, wr, tag):
    z, a = tmp["z"], tmp["a"]
    rz, ra = (tag, "z"), (tag, "a")
    if bias_is_col:
        S.act(z, src_ps, AF.Identity, rd, [rz], bias=bias_ap, scale=1.0)
    else:
        S.tt(DVE, z, src_ps, bias_ap, ALU.add, rd, [rz])
    S.act(a, z, AF.Square, [rz], [ra])
    S.ts(DVE, a, a, 0.044715, 1.0, ALU.mult, ALU.add, [ra], [ra])
    S.tt(DVE, a, a, z, ALU.mult, [ra, rz], [ra])
    S.act(a, a, AF.Sigmoid, [ra], [ra], scale=1.5957691216)
    S.tt(DVE, dst, a, z, ALU.mult, [ra, rz], wr)


def emit_front0(S, C, nc, NBA, dr, identf, identb):
    NBO = NBA // 2
    TA = NBA * 128
    NT = NBO * 128
    TW = min(512, NT)
    with ExitStack() as st0:
        ctok = C.sb(st0, [128, NBA, 8], F32, "ctok")
        nctok = C.sb(st0, [128, NBA, 8], F32, "nctok")
        chl = C.sb(st0, [40, TA], BF16, "chl")
        epsc = C.sb(st0, [128, 1], F32, "epsc0")
        S.add(POOL, lambda e: e.memset(epsc[:], LN_EPS), [], ["epsc"])
        S.add(POOL, lambda e: e.memset(chl[:], 0.0), [], ["chl"])
        lf = C.sb(st0, [128, NBA, 8], F32, "lf")
        with ExitStack() as st:
            xT = C.sb(st, [128, 16, NT], BF16, "xT")
            xl = [C.sb(st, [128, 2048], F32) for _ in range(2)]
            xb = [C.sb(st, [128, 2048], BF16) for _ in range(2)]
            ptp = [C.ps(st, [128, 8, 128], BF16) for _ in range(2)]
            wt = [C.sb(st, [128, 16, 128], BF16) for _ in range(3)]
            pj = [C.ps(st, [128, 512], F32) for _ in range(3)]
            gt = [dict(z=C.sb(st, [128, 512], F32), a=C.sb(st, [128, 512], F32)) for _ in range(2)]
            bau = C.sb(st, [128, 8], F32, "bau")
            S.dma(SP, bau[:], dr["bau"], [], ["bau"])
            ost = [C.sb(st, [128, 512], BF16) for _ in range(2)]
            wtm = C.sb(st, [128, 16, 1024], BF16, "wtm")
            bav = C.sb(st, [128, 1024], F32, "bav")
            gv = C.sb(st, [128, 1024], F32, "gv")
            bv = C.sb(st, [128, 1024], F32, "bv")
            S.dma(SP, bav[:], dr["bav"].partition_broadcast(128), [], ["bav"])
            S.dma(SP, gv[:], dr["gv"].partition_broadcast(128), [], ["lnconst"], key="gv")
            S.dma(SP, bv[:], dr["bv"].partition_broadcast(128), [], ["lnconst"], key="bv")
            vtile = [C.sb(st, [128, 1024], F32) for _ in range(2)]
            vst = [C.sb(st, [128, 1024], BF16) for _ in range(2)]
            lnt = [dict(st6=C.sb(st, [128, 4, 6], F32), mv=C.sb(st, [128, 2], F32), sq=C.sb(st, [128, 1], F32),
                        rstd=C.sb(st, [128, 1], F32), epsc=epsc) for _ in range(2)]
            wf = C.sb(st, [128, 16, 8], BF16, "wf")
            S.dma(POOL, wf[:], dr["wf"], [], ["wf"])
            bfb = C.sb(st, [128, 8], F32, "bfb")
            S.dma(SP, bfb[:], dr["bf"].partition_broadcast(128), [], ["bfb"])
            nw = 0
            npj = 0
            no = 0
            for hp in range(2):
                for bl in range(NBO):
                    b = hp * NBO + bl
                    i = b % 2
                    S.dma(SP, xl[i][:], dr["xall"][b * 128:(b + 1) * 128, :], [], [("xl", i)])
                    S.cp(ACT, xb[i][:], xl[i][:], [("xl", i)], [("xb", i)])
                    for hh in range(2):
                        for k in range(8):
                            kk = hh * 8 + k
                            S.tr(ptp[hh][:, k, :], xb[i][:, kk * 128:(kk + 1) * 128], identb[:], [("xb", i), "identb"],
                                 [("ptp", hh)])
                        S.cp(DVE, xT[:, hh * 8:(hh + 1) * 8, bl * 128:(bl + 1) * 128], ptp[hh][:], [("ptp", hh)],
                             [("xT", bl)])
                for kind in (("u", "q", "k") if hp == 0 else ("k",)):
                    for j in range(8):
                        wi = nw % 3
                        nw += 1
                        S.dma(POOL, wt[wi][:], dr["w" + kind][j], [], [("wt", wi)])
                        for t0 in range(0, NT, TW):
                            pi = npj % 3
                            npj += 1
                            oi = no % 2
                            no += 1
                            blks = [("xT", bb) for bb in range(t0 // 128, (t0 + TW) // 128)]
                            for k in range(16):
                                S.mm(pj[pi][:, 0:TW], wt[wi][:, k, :], xT[:, k, t0:t0 + TW], k == 0, k == 15,
                                     [("wt", wi)] + blks, [("pj", pi)])
                            if kind == "u":
                                g = gt[npj % 2]
                                emit_gelu(S, pj[pi][:, 0:TW], bau[:, j:j + 1], True, ost[oi][:, 0:TW],
                                          dict(z=g["z"][:, 0:TW], a=g["a"][:, 0:TW]), [("pj", pi), "bau"],
                                          [("ost", oi)], ("gt", npj % 2))
                                S.dma(SP, dr["uTd"][:, j, t0:t0 + TW], ost[oi][:, 0:TW], [("ost", oi)], [("uTd", j)],
                                      key=("ost_st", oi))
                            elif kind == "q":
                                S.act(ost[oi][:, 0:TW], pj[pi][:, 0:TW], AF.Copy, [("pj", pi)], [("ost", oi)],
                                      scale=SCALE)
                                S.dma(SP, dr["qTd"][:, j, t0:t0 + TW], ost[oi][:, 0:TW], [("ost", oi)], [("qTd", j)],
                                      key=("ost_st", oi))
                            else:
                                S.cp(ACT, ost[oi][:, 0:TW], pj[pi][:, 0:TW], [("pj", pi)], [("ost", oi)])
                                S.dma(SP, dr["kT"][:, j, hp * NT + t0:hp * NT + t0 + TW], ost[oi][:, 0:TW],
                                      [("ost", oi)], [("kT", j)], key=("ost_st", oi))
                if hp == 0:
                    for ch in range(2):
                        S.dma(POOL, wtm[:, :, ch * 512:(ch + 1) * 512], dr["wvs"][ch], [], [("wtm", ch)])
                    for bl in range(NBO):
                        i = bl % 2
                        for ch in range(2):
                            pi = npj % 3
                            npj += 1
                            for k in range(16):
                                S.mm(pj[pi][:], xT[:, k, bl * 128:(bl + 1) * 128], wtm[:, k, ch * 512:(ch + 1) * 512],
                                     k == 0, k == 15, [("xT", bl), ("wtm", ch)], [("pj", pi)])
                            g = gt[npj % 2]
                            emit_gelu(S, pj[pi][:], bav[:, ch * 512:(ch + 1) * 512], False,
                                      vtile[i][:, ch * 512:(ch + 1) * 512], dict(z=g["z"][:], a=g["a"][:]),
                                      [("pj", pi), "bav"], [("vtile", i)], ("gt", npj % 2))
                        emit_ln(S, C, vtile[i][:], vst[i][:], gv[:], bv[:], lnt[i], [("vtile", i)], [("vst", i)],
                                LN_EPS, "lnv_%d" % i, ncol=1024)
                        S.dma(SP, dr["vsd"][bl * 128:(bl + 1) * 128, :], vst[i][:], [("vst", i)], [("vsd", bl)],
                              key=("vst_st", i))
                for ch in range(2):
                    S.dma(POOL, wtm[:, :, ch * 512:(ch + 1) * 512], dr["wv"][ch], [], [("wtm", ch)])
                for bl in range(NBO):
                    b = hp * NBO + bl
                    i = bl % 2
                    for ch in range(2):
                        pi = npj % 3
                        npj += 1
                        for k in range(16):
                            S.mm(pj[pi][:], xT[:, k, bl * 128:(bl + 1) * 128], wtm[:, k, ch * 512:(ch + 1) * 512],
                                 k == 0, k == 15, [("xT", bl), ("wtm", ch)], [("pj", pi)])
                        S.cp(ACT if ch == 0 else DVE, vst[i][:, ch * 512:(ch + 1) * 512], pj[pi][:], [("pj", pi)],
                             [("vst", i)])
                    S.dma(SP, dr["V"][b * 128:(b + 1) * 128, :], vst[i][:], [("vst", i)], [("V", b)],
                          key=("vst_st", i))
                for bl in range(NBO):
                    b = hp * NBO + bl
                    pi = npj % 3
                    npj += 1
                    for k in range(16):
                        S.mm(pj[pi][:, 0:8], xT[:, k, bl * 128:(bl + 1) * 128], wf[:, k, :], k == 0, k == 15,
                             [("xT", bl), "wf"], [("pj", pi)])
                    S.tt(DVE, lf[:, b, :], pj[pi][:, 0:8], bfb[:], ALU.add, [("pj", pi), "bfb"], [("lf", b)])
        S.barrier()
        with ExitStack() as st:
            pj = [C.ps(st, [128, 512], F32) for _ in range(1)]
            lfall = [("lf", b) for b in range(NBA)]
            lf2 = lf[:].rearrange("p b h -> p (b h)")
            S.act(lf2, lf2, AF.Exp, lfall, lfall, scale=-1.0)
            S.act(lf2, lf2, AF.Ln, lfall, lfall, bias=1.0, scale=1.0)
            S.ts(DVE, lf2, lf2, -1.0, None, ALU.mult, ALU.bypass, lfall, lfall)
            utri = C.sb(st, [128, 128], F32, "utri")
            ones = C.sb(st, [128, 128], F32, "onesf")
            S.add(POOL, lambda e: e.memset(ones[:], 1.0), [], ["onesf"])
            S.add(POOL, lambda e: e.memset(utri[:], 1.0), [], ["utri"])
            S.add(POOL, lambda e: e.affine_select(utri[:], utri[:], [[1, 128]], ALU.is_ge, 0.0, base=0,
                                                  channel_multiplier=-1), ["utri"], ["utri"])
            hf = C.sb(st, [128, 2], F32, "hf")
            S.dma(SP, hf[:], dr["hf"], [], ["hf"])
            pc = C.ps(st, [128, 512], F32, "pc")
            ptot = C.ps(st, [128, 512], F32, "ptot")
            tot = C.sb(st, [128, 2, 8], F32, "tot")
            for half in range(2):
                for j in range(NBO):
                    S.mm(ptot[:, half * 8:half * 8 + 8], ones[:], lf[:, half * NBO + j, :], j == 0, j == NBO - 1,
                         ["onesf"] + lfall, ["ptot"])
            for half in range(2):
                S.cp(DVE, tot[:, half, :], ptot[:, half * 8:half * 8 + 8], ["ptot"], [("tot", half)])
            S.ts(DVE, tot[:, 1, :], tot[:, 1, :], hf[:, 0:1], None, ALU.mult, ALU.bypass, [("tot", 1), "hf"],
                 [("tot", 1)])
            S.ts(DVE, tot[:, 0, :], tot[:, 0, :], hf[:, 1:2], None, ALU.mult, ALU.bypass, [("tot", 0), "hf"],
                 [("tot", 0)])
            for half in range(2):
                for j in range(NBO):
                    b = half * NBO + j
                    for i2 in range(j + 1):
                        S.mm(pc[:, 0:8], utri[:] if i2 == j else ones[:], lf[:, half * NBO + i2, :], i2 == 0, i2 == j,
                             ["utri", "onesf"] + lfall, ["pc"])
                    S.tt(DVE, ctok[:, b, :], pc[:, 0:8], tot[:, 1 - half, :], ALU.add, ["pc", ("tot", 1 - half)],
                         [("ctok", b)])
            call = [("ctok", b) for b in range(NBA)]
            S.ts(DVE, nctok[:].rearrange("p b h -> p (b h)"), ctok[:].rearrange("p b h -> p (b h)"), -1.0, None,
                 ALU.mult, ALU.bypass, call, ["nctok"])
            pct = C.ps(st, [8, 512], F32, "pct")
            c32 = C.sb(st, [8, 512], F32, "c32")
            chi = C.sb(st, [8, 512], BF16, "chi")
            chf = C.sb(st, [8, 512], F32, "chf")
            clo = C.sb(st, [8, 512], BF16, "clo")
            for b0 in range(0, NBA, 4):
                nb = min(4, NBA - b0)
                for bb in range(nb):
                    S.tr(pct[:, bb * 128:(bb + 1) * 128], ctok[:, b0 + bb, :], identf[:], [("ctok", b0 + bb), "identf"],
                         ["pct"])
                w = nb * 128
                S.cp(DVE, c32[:, 0:w], pct[:, 0:w], ["pct"], ["c32"])
                S.cp(DVE, chl[0:8, b0 * 128:b0 * 128 + w], c32[:, 0:w], ["c32"], ["chl"])
                S.cp(DVE, chf[:, 0:w], chl[0:8, b0 * 128:b0 * 128 + w], ["chl"], ["chf"])
                S.tt(DVE, c32[:, 0:w], c32[:, 0:w], chf[:, 0:w], ALU.subtract, ["c32", "chf"], ["c32"])
                S.cp(DVE, clo[:, 0:w], c32[:, 0:w], ["c32"], ["clo"])
                S.dma(SP, chl[32:40, b0 * 128:b0 * 128 + w], clo[:, 0:w], ["clo"], ["chl"], key="clo_mv")
        S.barrier()
        with ExitStack() as st:
            uT = C.sb(st, [128, 8, NT], BF16, "uT")
            vsb = C.sb(st, [128, NBO, 1024], BF16, "vsb")
            for j in range(8):
                S.dma(SP, uT[:, j, :], dr["uTd"][:, j, :], [("uTd", j)], [("uT", j)], key="ld_uT")
            S.join([("uT", j) for j in range(8)])
            for b in range(NBO):
                S.dma(SP, vsb[:, b, :], dr["vsd"][b * 128:(b + 1) * 128, :], [("vsd", b)], [("vsb", b)], key="ld_vsb")
            S.join([("vsb", b) for b in range(NBO)])
            wsT = C.sb(st, [128, 8, 128], BF16, "wsT")
            S.dma(POOL, wsT[:], dr["wsT"], [], ["wsT"])
            for g in range(8):
                S.add(POOL, lambda e, g=g: e.affine_select(wsT[:, g, :], wsT[:, g, :], [[1, 128]], ALU.is_ge, 0.0,
                                                           base=0, channel_multiplier=-1), ["wsT"], ["wsT"])
            bsb = C.sb(st, [128, 8, 128], F32, "bsb")
            S.dma(SP, bsb[:].rearrange("p g t -> p (g t)"), dr["bs"].partition_broadcast(128), [], ["bsb"])
            psg = [C.ps(st, [128, 512], F32) for _ in range(2)]
            t1 = [C.sb(st, [128, 512], F32) for _ in range(2)]
            yo = [C.sb(st, [128, 512], BF16) for _ in range(2)]
            n = 0
            NG4 = TW // 128
            for g in range(8):
                for b0 in range(0, NBO, NG4):
                    i = n % 2
                    n += 1
                    for bb in range(NG4):
                        b = b0 + bb
                        S.mm(psg[i][:, bb * 128:(bb + 1) * 128], vsb[:, b, g * 128:(g + 1) * 128], wsT[:, g, :], True,
                             True, [("vsb", b), "wsT"], [("psg", i)])
                    for bb in range(NG4):
                        S.tt(DVE, t1[i][:, bb * 128:(bb + 1) * 128], psg[i][:, bb * 128:(bb + 1) * 128], bsb[:, g, :],
                             ALU.add, [("psg", i), "bsb"], [("t1", i)])
                    S.tt(DVE, yo[i][:, 0:TW], t1[i][:, 0:TW], uT[:, g, b0 * 128:b0 * 128 + TW], ALU.mult,
                         [("t1", i), ("uT", g)], [("yo", i)])
                    S.dma(SP, dr["yT"][:, g, b0 * 128:b0 * 128 + TW], yo[i][:, 0:TW], [("yo", i)],
                          [("yT", g, b0)], key=("yo_st", i))
        S.barrier()
        with ExitStack() as st:
            qT = C.sb(st, [128, 8, NT], BF16, "qT")
            for j in range(8):
                S.dma(SP, qT[:, j, :], dr["qTd"][:, j, :], [("qTd", j)], [("qT", j)], key="ld_qT")
            S.join([("qT", j) for j in range(8)])
            kT = C.sb(st, [128, 8, TA], BF16, "kTs")
            Vs = C.sb(st, [128, NBA, 1024], BF16, "Vs")
            for j in range(8):
                S.dma(SP, kT[:, j, :], dr["kT"][:, j, :], [("kT", j)], [("kTs", j)], key="ld_kTs")
            S.join([("kTs", j) for j in range(8)])
            for b in range(NBA):
                S.dma(SP, Vs[:, b, :], dr["V"][b * 128:(b + 1) * 128, :], [("V", b)], [("Vs", b)], key="ld_Vs")
            S.join([("Vs", b) for b in range(NBA)])
            sel = C.sb(st, [40, 8, 128], BF16, "sel")
            S.add(POOL, lambda e: e.memset(sel[:], 0.0), [], ["sel"])
            for off in (0, 32):
                S.add(POOL, lambda e, off=off: e.affine_select(sel[:], sel[:], [[-1, 8], [0, 128]], ALU.not_equal, 1.0,
                                                                 base=-off, channel_multiplier=1), ["sel"], ["sel"])
            onesb = C.sb(st, [128, 128], BF16, "onesb")
            S.add(POOL, lambda e: e.memset(onesb[:], 1.0), [], ["onesb"])
            mk = [C.sb(st, [128, NBA, 128], BF16) for _ in range(2)]
            pS = [C.ps(st, [128, 512], F32) for _ in range(3)]
            pO = [C.ps(st, [128, 512], F32) for _ in range(2)]
            pL = [C.ps(st, [128, 512], F32) for _ in range(2)]
            sS = [C.sb(st, [128, 512], F32) for _ in range(2)]
            pT = [C.sb(st, [128, 512], BF16) for _ in range(2)]
            rl = [C.sb(st, [128, 128], F32) for _ in range(2)]
            ob = [C.sb(st, [128, 128], BF16) for _ in range(2)]
            nS = 0
            nQ = 0
            for s in range(NBO):
                mi = s % 2
                S.dma(SP, mk[mi][:], dr["maskT"][s], [], [("mk", mi)])
                for hd in range(8):
                    oi = nQ % 2
                    nQ += 1
                    for c0 in range(0, NBA, 4):
                        nb = min(4, NBA - c0)
                        si = nS % 3
                        bi = nS % 2
                        nS += 1
                        for bb in range(nb):
                            kb = c0 + bb
                            S.mm(pS[si][:, bb * 128:(bb + 1) * 128], kT[:, hd, kb * 128:(kb + 1) * 128],
                                 qT[:, hd, s * 128:(s + 1) * 128], True, False, [("kTs", hd), ("qT", hd)],
                                 [("pS", si)])
                            S.mm(pS[si][:, bb * 128:(bb + 1) * 128], sel[:, hd, :], chl[:, s * 128:(s + 1) * 128],
                                 False, True, ["sel", "chl"], [("pS", si)])
                        for bb in range(nb):
                            kb = c0 + bb
                            S.add(DVE, lambda e, bi=bi, si=si, bb=bb, kb=kb, hd=hd, mi=mi: e.scalar_tensor_tensor(
                                sS[bi][:, bb * 128:(bb + 1) * 128], pS[si][:, bb * 128:(bb + 1) * 128],
                                nctok[:, kb, hd:hd + 1], mk[mi][:, kb, :], ALU.add, ALU.add),
                                  [("pS", si), "nctok", ("mk", mi)], [("sS", bi)])
                        w = nb * 128
                        S.act(pT[bi][:, 0:w], sS[bi][:, 0:w], AF.Exp, [("sS", bi)], [("pT", bi)])
                        for bb in range(nb):
                            kb = c0 + bb
                            S.mm(pO[oi][:, 0:128], Vs[:, kb, hd * 128:(hd + 1) * 128], pT[bi][:, bb * 128:(bb + 1) * 128],
                                 kb == 0, kb == NBA - 1, [("Vs", kb), ("pT", bi)], [("pO", oi)])
                            S.mm(pL[oi][:, 0:128], onesb[:], pT[bi][:, bb * 128:(bb + 1) * 128], kb == 0,
                                 kb == NBA - 1, ["onesb", ("pT", bi)], [("pL", oi)])
                    S.add(DVE, lambda e, oi=oi: e.reciprocal(rl[oi][:], pL[oi][:, 0:128]), [("pL", oi)], [("rl", oi)])
                    S.tt(DVE, ob[oi][:], pO[oi][:, 0:128], rl[oi][:], ALU.mult, [("pO", oi), ("rl", oi)], [("ob", oi)])
                    S.dma(SP, dr["yT"][:, 8 + hd, s * 128:(s + 1) * 128], ob[oi][:], [("ob", oi)], [("yT", 8 + hd, s)],
                          key=("ob_st", oi))
    S.barrier()


GN_EPS = 64e-5
RW_TW = int(os.environ.get('RW_TW', '512'))
RW_STOP = int(os.environ.get('RW_STOP', '9'))
RW_A1 = int(os.environ.get('RW_A1', '9'))
RW_NB = int(os.environ.get('RW_NB', '2'))
RW_A0 = int(os.environ.get('RW_A0', '9'))
RW_SUB = int(os.environ.get('RW_SUB', '9'))
RW_HP = int(os.environ.get('RW_HP', '2'))
EW = 0.6065306597126334


def emit_rwkv(S, C, nc, T, NCT, dr, identf, identb):
    NCHK = T // 64
    TW = min(RW_TW, T)
    NTT = T // TW
    CPT = TW // 64
    GC = 4
    NH = 2 * NCT
    with ExitStack() as st0:
        m_su = C.sb(st0, [64, 64], BF16, "m_su")
        m_ui = C.sb(st0, [64, 64], BF16, "m_ui")
        m_sl = C.sb(st0, [64, 64], BF16, "m_sl")
        for m, cm, base in ((m_su, -1, -1), (m_ui, -1, 0)):
            S.add(POOL, lambda e, m=m: e.memset(m[:], 1.0), [], ["masks"])
            S.add(POOL, lambda e, m=m, cm=cm, base=base: e.affine_select(m[:], m[:], [[1, 64]], ALU.is_ge, 0.0,
                                                                         base=base, channel_multiplier=cm), ["masks"],
                  ["masks"])
        S.add(POOL, lambda e: e.memset(m_sl[:], 1.0), [], ["masks"])
        S.add(POOL, lambda e: e.affine_select(m_sl[:], m_sl[:], [[-1, 64]], ALU.is_ge, 0.0, base=-1,
                                              channel_multiplier=1), ["masks"], ["masks"])
        bones = C.sb(st0, [128, 128], F32, "bones")
        S.add(POOL, lambda e: e.memset(bones[:], 0.0), [], ["bones"])
        S.add(POOL, lambda e: e.memset(bones[0:64, 0:64], 1.0), ["bones"], ["bones"])
        S.add(POOL, lambda e: e.memset(bones[64:128, 64:128], 1.0), ["bones"], ["bones"])
        bonesb = C.sb(st0, [128, 128], BF16, "bonesb")
        S.cp(DVE, bonesb[:], bones[:], ["bones"], ["bonesb"])
        smask = C.sb(st0, [128, TW], F32, "smask")
        S.add(POOL, lambda e: e.memset(smask[:], 1.0), [], ["smask"])
        S.add(POOL, lambda e: e.memset(smask[:].rearrange("p (c t) -> p c t", t=64)[:, :, 0:1], 0.0), ["smask"],
              ["smask"])
        cv = {}
        for nm in ("w0", "a0", "kk", "ka", "rk", "gng", "gnb"):
            cv[nm] = C.sb(st0, [128, NCT], F32, "cv_" + nm)
            S.dma(SP, cv[nm][:], dr[nm], [], [("cvr", nm)], key="ld_cv")
        S.join([("cvr", nm) for nm in ("w0", "a0", "kk", "ka", "rk", "gng", "gnb")] + ["cv"])
        mu = C.sb(st0, [128, 6, 16], F32, "mu")
        S.dma(SP, mu[:], dr["mu"], [], ["mu"])
        epsg = C.sb(st0, [128, 1], F32, "epsg")
        S.add(POOL, lambda e: e.memset(epsg[:], GN_EPS), [], ["epsg"])
        tiny = C.sb(st0, [128, 1], F32, "tiny")
        S.add(POOL, lambda e: e.memset(tiny[:], 1e-24), [], ["tiny"])
        with ExitStack() as st:
            wst = {n: [C.sb(st, [128, 16, 128], BF16, "W%s%d" % (n, i)) for i in range(2)] for n in ("r", "k", "v")}
            w1 = C.sb(st, [128, 16, 96], BF16, "w1")
            a1 = C.sb(st, [128, 16, 96], BF16, "a1")
            g1 = C.sb(st, [128, 16, 256], BF16, "g1")
            w2 = C.sb(st, [96, NCT * 128], BF16, "w2")
            a2 = C.sb(st, [96, NCT * 128], BF16, "a2")
            g2 = C.sb(st, [128, 2, NCT * 128], BF16, "g2")
            for t_, nm in ((w1, "w1"), (a1, "a1"), (g1, "g1"), (w2, "w2"), (a2, "a2"), (g2, "g2")):
                S.dma(POOL, t_[:], dr[nm], [], [nm], key="ld_lora")
            S.join(["w1", "a1", "g1", "w2", "a2", "g2"])
            xl = [C.sb(st, [128, 2048], F32) for _ in range(1)]
            xb = [C.sb(st, [128, 2048], BF16) for _ in range(1)]
            xT = C.sb(st, [128, 16, TW + 1], BF16, "xT")
            dx = C.sb(st, [128, 16, TW], BF16, "dx")
            xm = [C.sb(st, [128, 16, TW], BF16) for _ in range(2)]
            S.add(POOL, lambda e: e.memset(xT[:, :, 0:1], 0.0), [], ["xTprev"])
            ptp = [C.ps(st, [128, 8, 128], BF16) for _ in range(2)]
            pj = [C.ps(st, [128, 512], F32) for _ in range(3)]
            plo = C.ps(st, [128, 512], F32, "plo")
            hlo = {n: C.sb(st, [128, TW], BF16, "h" + n) for n in ("w", "a")}
            hg = C.sb(st, [128, 2, TW], BF16, "hg")
            F = lambda nm: [C.sb(st, [128, TW], F32, "%s%d" % (nm, i)) for i in range(2)]
            tr_, tk_, tv_, ta_, tb_, tld, tlc, tt1, tt2 = F("tr"), F("tk"), F("tv"), F("ta"), F("tb"), F("tld"), F("tlc"), F("tt1"), F("tt2")
            tkk, tkm = F("tkk"), F("tkm")
            ob = {n: [C.sb(st, [128, TW], BF16, "o%s%d" % (n, i)) for i in range(2)] for n in
                  ("B", "K", "Bh", "Kh", "v", "g")}
            oar = [C.sb(st, [128, CPT, 128], BF16, "oar%d" % i) for i in range(2)]
            wend = [C.sb(st, [128, CPT], F32, "wend%d" % i) for i in range(2)]
            npj = 0
            nu = 0
            for tt in range(NTT):
                t0 = tt * TW
                if tt > 0:
                    S.cp(DVE, xT[:, :, 0:1], xT[:, :, TW:TW + 1], [("xTt", b) for b in range(TW // 128)], ["xTprev"])
                for bl in range(TW // 128):
                    b = tt * (TW // 128) + bl
                    i = 0
                    S.dma(SP, xl[i][:], dr["x"][b * 128:(b + 1) * 128, :], [], [("xl", i)])
                    S.cp(ACT, xb[i][:], xl[i][:], [("xl", i)], [("xb", i)])
                    for hh in range(2):
                        for k in range(8):
                            kk_ = hh * 8 + k
                            S.tr(ptp[hh][:, k, :], xb[i][:, kk_ * 128:(kk_ + 1) * 128], identb[:], [("xb", i), "identb"],
                                 [("ptp", hh)])
                        S.cp(DVE, xT[:, hh * 8:(hh + 1) * 8, 1 + bl * 128:1 + (bl + 1) * 128], ptp[hh][:],
                             [("ptp", hh)], [("xTt", bl)])
                xall = [("xTt", b) for b in range(TW // 128)] + ["xTprev"]
                S.tt(DVE, dx[:], xT[:, :, 0:TW], xT[:, :, 1:TW + 1], ALU.subtract, xall, ["dx"])

                def mix(n, slot):
                    for k in range(16):
                        S.add(DVE, lambda e, k=k: e.scalar_tensor_tensor(xm[slot][:, k, :], dx[:, k, :], mu[:, n, k:k + 1],
                                                                         xT[:, k, 1:TW + 1], ALU.mult, ALU.add),
                              ["dx", "mu"] + xall, [("xm", slot, k)])
                    return [("xm", slot, k) for k in range(16)]

                def proj(lhs_of_k, M, rd_w, rd_x, slot, out_ps):
                    for k in range(16):
                        S.mm(out_ps, lhs_of_k(k), xm[slot][:, k, :], k == 0, k == 15, rd_w + rd_x, [("pjx", id(out_ps))])

                rdx = mix(3, 0)
                for k in range(16):
                    S.mm(plo[0:96, 0:TW], w1[:, k, :], xm[0][:, k, :], k == 0, k == 15, ["w1"] + rdx, ["plo"])
                S.act(hlo["w"][0:96, :], plo[0:96, 0:TW], AF.Tanh, ["plo"], ["hw"])
                rdx = mix(4, 1)
                for k in range(16):
                    S.mm(plo[0:96, 0:TW], a1[:, k, :], xm[1][:, k, :], k == 0, k == 15, ["a1"] + rdx, ["plo"])
                S.cp(ACT, hlo["a"][0:96, :], plo[0:96, 0:TW], ["plo"], ["ha"])
                rdx = mix(5, 0)
                for mt in range(2):
                    for k in range(16):
                        S.mm(plo[:, 0:TW], g1[:, k, mt * 128:(mt + 1) * 128], xm[0][:, k, :], k == 0, k == 15,
                             ["g1"] + rdx, ["plo"])
                    S.act(hg[:, mt, :], plo[:, 0:TW], AF.Sigmoid, ["plo"], [("hg", mt)])
                rd_r = mix(0, 1)
                rd_k = mix(1, 0)
                for ct in range(NCT):
                    i = nu % 2
                    nu += 1
                    cs = slice(ct * 128, (ct + 1) * 128)
                    R = lambda nm: (nm, i)
                    S.dma(POOL, wst["r"][i][:], dr["wrkv"][0, ct], [], [("Wr", i)])
                    S.dma(POOL, wst["k"][i][:], dr["wrkv"][1, ct], [], [("Wk", i)])
                    pi = npj % 3; npj += 1
                    for k in range(16):
                        S.mm(pj[pi][:, 0:TW], wst["r"][i][:, k, :], xm[1][:, k, :], k == 0, k == 15, [("Wr", i)] + rd_r,
                             [("pj", pi)])
                    S.cp(ACT, tr_[i][:], pj[pi][:, 0:TW], [("pj", pi)], [R("tr")])
                    pi = npj % 3; npj += 1
                    for k in range(16):
                        S.mm(pj[pi][:, 0:TW], wst["k"][i][:, k, :], xm[0][:, k, :], k == 0, k == 15, [("Wk", i)] + rd_k,
                             [("pj", pi)])
                    S.cp(ACT, tk_[i][:], pj[pi][:, 0:TW], [("pj", pi)], [R("tk")])
                    pi = npj % 3; npj += 1
                    S.mm(pj[pi][:, 0:TW], w2[:, cs], hlo["w"][0:96, :], True, True, ["w2", "hw"], [("pj", pi)])
                    S.act(tld[i][:], pj[pi][:, 0:TW], AF.Sigmoid, [("pj", pi), "cv"], [R("tld")], bias=cv["w0"][:, ct:ct + 1],
                          scale=1.0)
                    S.ts(DVE, tld[i][:], tld[i][:], -EW, None, ALU.mult, ALU.bypass, [R("tld")], [R("tld")])
                    pi = npj % 3; npj += 1
                    S.mm(pj[pi][:, 0:TW], a2[:, cs], hlo["a"][0:96, :], True, True, ["a2", "ha"], [("pj", pi)])
                    S.act(ta_[i][:], pj[pi][:, 0:TW], AF.Sigmoid, [("pj", pi), "cv"], [R("ta")], bias=cv["a0"][:, ct:ct + 1],
                          scale=1.0)
                    pi = npj % 3; npj += 1
                    for mt in range(2):
                        S.mm(pj[pi][:, 0:TW], g2[:, mt, cs], hg[:, mt, :], mt == 0, mt == 1, ["g2", ("hg", mt)],
                             [("pj", pi)])
                    S.cp(ACT, ob["g"][i][:], pj[pi][:, 0:TW], [("pj", pi)], [R("og")])
                    S.dma(SP, dr["gT"][:, ct, t0:t0 + TW], ob["g"][i][:], [R("og")], [("gT", ct, tt)], key=R("og_st"))
                    S.ts(DVE, tkk[i][:], tk_[i][:], cv["kk"][:, ct:ct + 1], None, ALU.mult, ALU.bypass, [R("tk"), "cv"],
                         [R("tkk")])
                    S.tt(DVE, tt1[i][:], tkk[i][:], tkk[i][:], ALU.mult, [R("tkk")], [R("tt1")])
                    pi = npj % 3; npj += 1
                    S.mm(pj[pi][:, 0:TW], bones[:], tt1[i][:], True, True, ["bones", R("tt1")], [("pj", pi)])
                    S.act(tt1[i][:], pj[pi][:, 0:TW], AF.Sqrt, [("pj", pi), "tiny"], [R("tt1")], bias=tiny[:], scale=1.0)
                    S.add(DVE, lambda e, i=i: e.reciprocal(tt1[i][:], tt1[i][:]), [R("tt1")], [R("tt1")])
                    S.tt(DVE, tkk[i][:], tkk[i][:], tt1[i][:], ALU.mult, [R("tkk"), R("tt1")], [R("tkk")])
                    S.tt(DVE, tb_[i][:], tkk[i][:], ta_[i][:], ALU.mult, [R("tkk"), R("ta")], [R("tb")])
                    S.ts(DVE, tt2[i][:], ta_[i][:], -1.0, cv["ka"][:, ct:ct + 1], ALU.add, ALU.mult, [R("ta"), "cv"],
                         [R("tt2")])
                    S.add(DVE, lambda e, i=i: e.scalar_tensor_tensor(tkm[i][:], tt2[i][:], 1.0, tk_[i][:], ALU.add,
                                                                     ALU.mult), [R("tt2"), R("tk")], [R("tkm")])
                    S.add(DVE, lambda e, i=i: e.tensor_tensor_scan(tlc[i][:], smask[:], tld[i][:], 0.0, ALU.mult,
                                                                   ALU.add), ["smask", R("tld")], [R("tlc")])
                    lc3 = tlc[i][:].rearrange("p (c t) -> p c t", t=64)
                    S.act(tt1[i][:], tlc[i][:], AF.Exp, [R("tlc")], [R("tt1")])
                    S.tt(DVE, oar[i][:, :, 64:128], tr_[i][:].rearrange("p (c t) -> p c t", t=64),
                         tt1[i][:].rearrange("p (c t) -> p c t", t=64), ALU.mult, [R("tr"), R("tt1")], [R("oarR")])
                    S.add(DVE, lambda e, i=i, lc3=lc3: e.tensor_copy(wend[i][:], lc3[:, :, 63]), [R("tt1"), R("tlc")], [R("wend")])
                    S.tt(DVE, tt2[i][:], tlc[i][:], tld[i][:], ALU.subtract, [R("tlc"), R("tld")], [R("tt2")])
                    S.act(tt2[i][:], tt2[i][:], AF.Exp, [R("tt2")], [R("tt2")])
                    S.add(DVE, lambda e, i=i: e.scalar_tensor_tensor(oar[i][:, :, 0:64],
                                                                     tkk[i][:].rearrange("p (c t) -> p c t", t=64), -1.0,
                                                                     tt2[i][:].rearrange("p (c t) -> p c t", t=64),
                                                                     ALU.mult, ALU.mult), [R("tkk"), R("tt2")], [R("oarA")])
                    S.act(tt1[i][:], tlc[i][:], AF.Exp, [R("tlc"), R("oarR")], [R("tt1")], scale=-1.0)
                    S.tt(DVE, ob["B"][i][:], tb_[i][:], tt1[i][:], ALU.mult, [R("tb"), R("tt1")], [R("oB")])
                    S.tt(DVE, ob["K"][i][:], tkm[i][:], tt1[i][:], ALU.mult, [R("tkm"), R("tt1")], [R("oK")])
                    S.tt(DVE, tt2[i][:].rearrange("p (c t) -> p c t", t=64),
                         wend[i][:].unsqueeze(2).to_broadcast([128, CPT, 64]), lc3, ALU.subtract,
                         [R("wend"), R("tlc"), R("oarA")], [R("tt2")])
                    S.act(tt2[i][:], tt2[i][:], AF.Exp, [R("tt2")], [R("tt2")])
                    S.tt(DVE, ob["Bh"][i][:], tb_[i][:], tt2[i][:], ALU.mult, [R("tb"), R("tt2")], [R("oBh")])
                    S.tt(DVE, ob["Kh"][i][:], tkm[i][:], tt2[i][:], ALU.mult, [R("tkm"), R("tt2")], [R("oKh")])
                    S.act(wend[i][:], wend[i][:], AF.Exp, [R("wend"), R("tt2")], [R("wend")])
                    S.dma(SP, dr["ARt"][:, ct, tt * CPT:(tt + 1) * CPT, :], oar[i][:], [R("oarA"), R("oarR")],
                          [("ARt", ct, tt)], key=R("oar_st"))
                    for nm in ("B", "K", "Bh", "Kh"):
                        S.dma(SP, dr[nm + "t"][:, ct, t0:t0 + TW], ob[nm][i][:], [R("o" + nm)], [(nm + "t", ct, tt)],
                              key=R("o%s_st" % nm))
                    S.dma(SP, dr["wend"][:, ct, tt * CPT:(tt + 1) * CPT], wend[i][:], [R("wend")], [("wendd", ct, tt)],
                          key=R("wend_st"))
                    S.dma(SP, dr["r32"][:, ct, t0:t0 + TW], tr_[i][:], [R("tr")], [("r32", ct, tt)], key=R("r32_st"))
                    S.dma(SP, dr["k32"][:, ct, t0:t0 + TW], tkm[i][:], [R("tkm")], [("k32", ct, tt)], key=R("k32_st"))
                rd_v = mix(2, 1)
                for ct in range(NCT):
                    i = nu % 2
                    nu += 1
                    cs = slice(ct * 128, (ct + 1) * 128)
                    pi = npj % 3; npj += 1
                    S.dma(POOL, wst["v"][i][:], dr["wrkv"][2, ct], [], [("Wv", i)])
                    for k in range(16):
                        S.mm(pj[pi][:, 0:TW], wst["v"][i][:, k, :], xm[1][:, k, :], k == 0, k == 15, [("Wv", i)] + rd_v,
                             [("pj", pi)])
                    S.cp(ACT, tv_[i][:], pj[pi][:, 0:TW], [("pj", pi)], [("tv", i)])
                    S.cp(DVE, ob["v"][i][:], tv_[i][:], [("tv", i)], [("ov", i)])
                    S.dma(SP, dr["v32"][:, ct, t0:t0 + TW], tv_[i][:], [("tv", i)], [("v32", ct, tt)], key=("v32_st", i))
                    S.dma(SP, dr["vt"][:, ct, t0:t0 + TW], ob["v"][i][:], [("ov", i)], [("vt", ct, tt)],
                          key=("ov_st", i))
        S.barrier()
        if RW_STOP <= 0:
            return
        NG = NCHK // GC
        U = 2 * GC
        with ExitStack() as st:
            fm = {n: [C.sb(st, [64, 2, GC * 64], BF16, "fm%s%d" % (n, i)) for i in range(2)] for n in
                  ("B", "K", "Bh", "Kh", "v")}
            far = [C.sb(st, [64, 2, GC, 128], BF16, "far%d" % i) for i in range(2)]
            pA1 = C.ps(st, [64, U, 64], F32, "pA1")
            pA2 = C.ps(st, [64, U, 128], F32, "pA2")
            pB = C.ps(st, [64, U, 128], F32, "pB")
            pC = C.ps(st, [64, U, 64], F32, "pC")
            pT = C.ps(st, [64, GC, 4, 128], BF16, "pT")
            N_ = [C.sb(st, [64, U, 64], BF16, "N%d" % i) for i in range(2)]
            LY = [C.sb(st, [64, U, 192], BF16, "LY%d" % i) for i in range(2)]
            BM = C.sb(st, [64, U, 128], BF16, "BM")
            AKt = C.sb(st, [64, U, 64], BF16, "AKt")
            MRKt = C.sb(st, [64, U, 64], BF16, "MRKt")
            Vt = C.sb(st, [64, U, 64], BF16, "Vt")
            Kh = C.sb(st, [64, U, 64], BF16, "Kht")
            oGT = C.sb(st, [64, U, 64], BF16, "oGT")
            oRp = C.sb(st, [64, U, 64], BF16, "oRp")
            oY0 = C.sb(st, [64, U, 64], F32, "oY0")
            oH = C.sb(st, [64, U, 64], F32, "oH")
            bc = lambda m: m[:].unsqueeze(1).to_broadcast([64, U, 64])
            v4 = lambda t, lo, hi: t[:, :, lo:hi].rearrange("p (c h) w -> p c h w", h=2)
            ng = 0
            for ct in range(NCT):
                for g in range(NG):
                    i = ng % RW_NB
                    ng += 1
                    c0 = g * GC
                    tt = (c0 * 64) // TW
                    R = lambda nm: (nm, i)
                    for nm in ("B", "K", "Bh", "Kh", "v"):
                        S.dma(SP, fm[nm][i][:], dr[nm + "t"][:, ct, c0 * 64:(c0 + GC) * 64].rearrange(
                            "(h p) c -> p h c", h=2), [(nm + "t", ct, tt)], [R("fm" + nm)])
                    S.dma(SP, far[i][:], dr["ARt"][:, ct, c0:c0 + GC, :].rearrange("(h p) c w -> p h c w", h=2),
                          [("ARt", ct, tt)], [R("far")])
                    lvl = RW_A1 if g >= 1 else RW_A0
                    if lvl <= 1:
                        continue
                    ua = lambda t, cc, hp: t[:, hp, cc * 64:(cc + 1) * 64]
                    for cc in range(GC):
                        for hp in range(RW_HP):
                            u = cc * 2 + hp
                            ps = slice(hp * 64, (hp + 1) * 64)
                            S.mm(pA2[:, u, :], ua(fm["B"][i], cc, hp), far[i][:, hp, cc, :], True, True,
                                 [R("fmB"), R("far")], ["pA2"])
                            S.mm(pB[:, u, :], ua(fm["K"][i], cc, hp), far[i][:, hp, cc, :], True, True,
                                 [R("fmK"), R("far")], ["pB"])
                            S.mm(pC[:, u, :], far[i][:, hp, cc, 0:64], ua(fm["B"][i], cc, hp), True, True,
                                 [R("fmB"), R("far")], ["pC"])
                    for cc in range(GC if RW_SUB >= 2 else 0):
                        for hp in range(2):
                            hs = slice(hp * 64, (hp + 1) * 64)
                            idb = identb[0:64, 0:64]
                            S.tr(pT[:, cc, 0, hs], far[i][:, hp, cc, 0:64], idb, [R("far"), "identb"], ["pT"])
                            S.tr(pT[:, cc, 1, hs], ua(fm["Bh"][i], cc, hp), idb, [R("fmBh"), "identb"], ["pT"])
                            S.tr(pT[:, cc, 2, hs], ua(fm["Kh"][i], cc, hp), idb, [R("fmKh"), "identb"], ["pT"])
                            S.tr(pT[:, cc, 3, hs], ua(fm["v"][i], cc, hp), idb, [R("fmv"), "identb"], ["pT"])
                    tsrc = lambda k: pT[:, :, k, :].rearrange("p c (h w) -> p c h w", h=2)
                    if RW_SUB >= 3:
                        S.cp(ACT, v4(LY[0], 64, 128), tsrc(0), ["pT"], ["LY0"])
                        S.cp(ACT, v4(BM, 0, 64), tsrc(1), ["pT"], ["BMb"])
                        S.cp(ACT, v4(Kh, 0, 64), tsrc(2), ["pT"], ["Kht"])
                        S.cp(ACT, v4(Vt, 0, 64), tsrc(3), ["pT"], ["Vt"])
                    if RW_SUB <= 3:
                        continue
                    S.tt(DVE, N_[0][:], pA2[:, :, 0:64], bc(m_su), ALU.mult, ["pA2", "masks"], ["N0"])
                    S.tt(DVE, BM[:, :, 64:128], pA2[:, :, 64:128], bc(m_ui), ALU.mult, ["pA2", "masks"], ["BMm"])
                    S.tt(DVE, AKt[:], pB[:, :, 0:64], bc(m_su), ALU.mult, ["pB", "masks"], ["AKt"])
                    S.tt(DVE, MRKt[:], pB[:, :, 64:128], bc(m_ui), ALU.mult, ["pB", "masks"], ["MRKt"])
                    S.tt(DVE, LY[0][:, :, 0:64], pC[:], bc(m_sl), ALU.mult, ["pC", "masks"], ["LY0"])
                    for u in range(U):
                        S.mm(pC[:, u, :], AKt[:, u, :], Vt[:, u, :], True, True, ["AKt", "Vt"], ["pC"])
                    S.cp(DVE, LY[0][:, :, 128:192], pC[:], ["pC"], ["LY0"])
                    if lvl <= 2:
                        continue
                    for j in range(6):
                        a, b = j % 2, (j + 1) % 2
                        last = j == 5
                        for u in range(U):
                            S.mm(pA2[:, u, :], N_[a][:, u, :], LY[a][:, u, 64:192], True, True,
                                 ["N%d" % a, "LY%d" % a], ["pA2"])
                            if not last:
                                S.mm(pA1[:, u, :], N_[a][:, u, :], LY[a][:, u, 0:64], True, True,
                                     ["N%d" % a, "LY%d" % a], ["pA1"])
                                S.mm(pC[:, u, :], LY[a][:, u, 0:64], N_[a][:, u, :], True, True,
                                     ["N%d" % a, "LY%d" % a], ["pC"])
                        S.tt(DVE, LY[b][:, :, 64:192], pA2[:], LY[a][:, :, 64:192], ALU.add,
                             ["pA2", "LY%d" % a], ["LY%d" % b])
                        if not last:
                            S.cp(ACT, LY[b][:, :, 0:64], pA1[:], ["pA1"], ["LY%d" % b])
                            S.cp(ACT, N_[b][:], pC[:], ["pC"], ["N%d" % b])
                    Yf = LY[0]
                    if lvl <= 3:
                        continue
                    for u in range(U):
                        S.mm(pB[:, u, :], Yf[:, u, 64:128], BM[:, u, :], True, True, ["LY0", "BMb", "BMm"], ["pB"])
                        S.mm(pA1[:, u, :], BM[:, u, 64:128], Yf[:, u, 128:192], True, False, ["LY0", "BMm"], ["pA1"])
                        S.mm(pA1[:, u, :], MRKt[:, u, :], Vt[:, u, :], False, True, ["MRKt", "Vt"], ["pA1"])
                        S.mm(pC[:, u, :], BM[:, u, 0:64], Yf[:, u, 128:192], True, False, ["LY0", "BMb"], ["pC"])
                        S.mm(pC[:, u, :], Kh[:, u, :], Vt[:, u, :], False, True, ["Kht", "Vt"], ["pC"])
                    S.cp(DVE, oGT[:], pB[:, :, 0:64], ["pB"], ["oGT"])
                    for hp in range(2):
                        S.tt(DVE, oRp[:].rearrange("p (c h) w -> p c h w", h=2)[:, :, hp, :],
                             pB[:, :, 64:128].rearrange("p (c h) w -> p c h w", h=2)[:, :, hp, :],
                             far[i][:, hp, :, 64:128], ALU.add, ["pB", R("far")], ["oRp"])
                    S.cp(ACT, oY0[:], pA1[:], ["pA1"], ["oY0"])
                    S.cp(DVE, oH[:], pC[:], ["pC"], ["oH"])
                    if lvl <= 4:
                        continue
                    osl = lambda d: d[c0:c0 + GC, :, 2 * ct:2 * ct + 2, :].rearrange("c p h w -> p c h w")
                    S.dma(SP, osl(dr["GT"]), oGT[:].rearrange("p (c h) w -> p c h w", h=2), ["oGT"], [("GT", ct, g)],
                          key="oGT_st")
                    S.dma(SP, osl(dr["RpT"]), oRp[:].rearrange("p (c h) w -> p c h w", h=2), ["oRp"], [("RpT", ct, g)],
                          key="oRp_st")
                    S.dma(SP, osl(dr["Y0p"]), oY0[:].rearrange("p (c h) w -> p c h w", h=2), ["oY0"], [("Y0p", ct, g)],
                          key="oY0_st")
                    S.dma(SP, osl(dr["Hm"]), oH[:].rearrange("p (c h) w -> p c h w", h=2), ["oH"], [("Hm", ct, g)],
                          key="oH_st")
        S.barrier()
        if RW_STOP <= 1:
            return
        with ExitStack() as st:
            wendS = C.sb(st, [64, NH, NCHK], F32, "wendS")
            for ct in range(NCT):
                for hp in range(2):
                    S.dma(SP, wendS[:, 2 * ct + hp, :], dr["wend"][hp * 64:(hp + 1) * 64, ct, :],
                          [("wendd", ct, tt) for tt in range(NTT)], [("wendSr", ct, hp)], key="ld_wendS")
            S.join([("wendSr", ct, hp) for ct in range(NCT) for hp in range(2)] + ["wendS"])
            P32 = C.sb(st, [64, NH, 64], F32, "P32")
            Pb = C.sb(st, [64, NH, 64], BF16, "Pb")
            S.add(POOL, lambda e: e.memset(P32[:], 0.0), [], ["P32"])
            S.add(POOL, lambda e: e.memset(Pb[:], 0.0), [], ["Pb"])
            gt_ = [C.sb(st, [64, NH, 64], BF16, "gt%d" % i) for i in range(2)]
            rp_ = [C.sb(st, [64, NH, 64], BF16, "rp%d" % i) for i in range(2)]
            y0_ = [C.sb(st, [64, NH, 64], F32, "y0%d" % i) for i in range(2)]
            hm_ = [C.sb(st, [64, NH, 64], F32, "hm%d" % i) for i in range(2)]
            yo_ = [C.sb(st, [64, NH * 64], F32, "yo%d" % i) for i in range(2)]
            pY = C.ps(st, [64, NH, 64], F32, "pY")
            pP = C.ps(st, [64, NH, 64], F32, "pP")
            pyt = [C.ps(st, [128, 512], F32) for _ in range(2)]
            yT = [C.sb(st, [128, NCT, 64], F32, "yTc%d" % i) for i in range(2)]
            allA = lambda nm, c: [(nm, ct, c // GC) for ct in range(NCT)]
            for c in range(NCHK):
                i = c % 2
                R = lambda nm: (nm, i)
                S.dma(SP, gt_[i][:], dr["GT"][c], allA("GT", c), [R("gt")])
                S.dma(SP, rp_[i][:], dr["RpT"][c], allA("RpT", c), [R("rp")])
                S.dma(SP, y0_[i][:], dr["Y0p"][c], allA("Y0p", c), [R("y0")])
                S.dma(SP, hm_[i][:], dr["Hm"][c], allA("Hm", c), [R("hm")])
                for h in range(NH):
                    S.mm(pY[:, h, :], rp_[i][:, h, :], Pb[:, h, :], True, True, [R("rp"), "Pb"], ["pY"])
                for h in range(NH):
                    S.mm(pP[:, h, :], gt_[i][:, h, :], Pb[:, h, :], True, True, [R("gt"), "Pb"], ["pP"])
                S.tt(DVE, yo_[i][:].rearrange("p (h w) -> p h w", w=64), pY[:], y0_[i][:], ALU.add, ["pY", R("y0")],
                     [R("yo")])
                S.tt(DVE, P32[:], P32[:], wendS[:, :, c:c + 1].to_broadcast([64, NH, 64]), ALU.mult,
                     ["P32", "wendS"], ["P32"])
                S.tt(DVE, P32[:], P32[:], pP[:], ALU.add, ["P32", "pP"], ["P32"])
                S.tt(DVE, P32[:], P32[:], hm_[i][:], ALU.add, ["P32", R("hm")], ["P32"])
                S.cp(ACT, Pb[:], P32[:], ["P32"], ["Pb"])
                for ct in range(NCT):
                    S.tr(pyt[i][:, ct * 64:(ct + 1) * 64], yo_[i][:, ct * 128:(ct + 1) * 128], identf[0:64, 0:64],
                         [R("yo"), "identf"], [("pyt", i)])
                S.cp(ACT, yT[i][:].rearrange("p c t -> p (c t)"), pyt[i][:, 0:NCT * 64], [("pyt", i)], [R("yTc")])
                S.dma(SP, dr["y32"][:, :, c * 64:(c + 1) * 64], yT[i][:], [R("yTc")], [("y32", c)], key=R("yTc_st"))
        S.barrier()
        if RW_STOP <= 2:
            return
        with ExitStack() as st:
            F = lambda nm: [C.sb(st, [128, TW], F32, "%s%d" % (nm, i)) for i in range(2)]
            c_y, c_r, c_k, c_v, c_m, c_q, c_s = F("c_y"), F("cr"), F("ck"), F("cv"), F("cm"), F("cq"), F("cs")
            c_g = [C.sb(st, [128, TW], BF16, "cg%d" % i) for i in range(2)]
            c_o = [C.sb(st, [128, TW], BF16, "co%d" % i) for i in range(2)]
            pm = [C.ps(st, [128, 512], F32) for _ in range(2)]
            pq = [C.ps(st, [128, 512], F32) for _ in range(2)]
            pb_ = [C.ps(st, [128, 512], F32) for _ in range(2)]
            n = 0
            for ct in range(NCT):
                for tt in range(NTT):
                    i = n % 2
                    n += 1
                    t0 = tt * TW
                    R = lambda nm: (nm, i)
                    S.dma(SP, c_y[i][:], dr["y32"][:, ct, t0:t0 + TW], [("y32", c) for c in range(tt * CPT, (tt + 1) * CPT)],
                          [R("c_y")])
                    S.dma(SP, c_r[i][:], dr["r32"][:, ct, t0:t0 + TW], [("r32", ct, tt)], [R("cr")])
                    S.dma(SP, c_k[i][:], dr["k32"][:, ct, t0:t0 + TW], [("k32", ct, tt)], [R("ck")])
                    S.dma(SP, c_v[i][:], dr["v32"][:, ct, t0:t0 + TW], [("v32", ct, tt)], [R("cv")])
                    S.dma(SP, c_g[i][:], dr["gT"][:, ct, t0:t0 + TW], [("gT", ct, tt)], [R("cg")])
                    S.mm(pm[i][:, 0:TW], bones[:], c_y[i][:], True, True, ["bones", R("c_y")], [("pm", i)])
                    S.tt(DVE, c_q[i][:], c_y[i][:], c_y[i][:], ALU.mult, [R("c_y")], [R("cq")])
                    S.mm(pq[i][:, 0:TW], bones[:], c_q[i][:], True, True, ["bones", R("cq")], [("pq", i)])
                    S.ts(DVE, c_m[i][:], pm[i][:, 0:TW], 1.0 / 64, None, ALU.mult, ALU.bypass, [("pm", i)], [R("cm")])
                    S.tt(DVE, c_s[i][:], c_m[i][:], c_m[i][:], ALU.mult, [R("cm")], [R("cs")])
                    S.add(DVE, lambda e, i=i: e.scalar_tensor_tensor(c_s[i][:], pq[i][:, 0:TW], 1.0 / 64, c_s[i][:],
                                                                     ALU.mult, ALU.subtract), [("pq", i), R("cs")],
                          [R("cs")])
                    S.act(c_s[i][:], c_s[i][:], AF.Sqrt, [R("cs"), "epsg"], [R("cs")], bias=epsg[:], scale=1.0)
                    S.add(DVE, lambda e, i=i: e.reciprocal(c_s[i][:], c_s[i][:]), [R("cs")], [R("cs")])
                    S.tt(DVE, c_y[i][:], c_y[i][:], c_m[i][:], ALU.subtract, [R("c_y"), R("cm")], [R("c_y")])
                    S.tt(DVE, c_y[i][:], c_y[i][:], c_s[i][:], ALU.mult, [R("c_y"), R("cs")], [R("c_y")])
                    S.ts(DVE, c_y[i][:], c_y[i][:], cv["gng"][:, ct:ct + 1], cv["gnb"][:, ct:ct + 1], ALU.mult, ALU.add,
                         [R("c_y"), "cv"], [R("c_y")])
                    S.add(DVE, lambda e, i=i, ct=ct: e.scalar_tensor_tensor(c_q[i][:], c_r[i][:], cv["rk"][:, ct:ct + 1],
                                                                            c_k[i][:], ALU.mult, ALU.mult),
                          [R("cr"), R("ck"), "cv", ("pq", i)], [R("cq")])
                    S.mm(pb_[i][:, 0:TW], bones[:], c_q[i][:], True, True, ["bones", R("cq")], [("pb", i)])
                    S.tt(DVE, c_q[i][:], pb_[i][:, 0:TW], c_v[i][:], ALU.mult, [("pb", i), R("cv")], [R("cq")])
                    S.tt(DVE, c_y[i][:], c_y[i][:], c_q[i][:], ALU.add, [R("c_y"), R("cq")], [R("c_y")])
                    S.tt(DVE, c_o[i][:], c_y[i][:], c_g[i][:], ALU.mult, [R("c_y"), R("cg")], [R("co")])
                    S.dma(SP, dr["ygT"][:, ct, t0:t0 + TW], c_o[i][:], [R("co")], [("ygT", ct, tt)], key=R("co_st"))


def front0_drams(nc, NBA, SCRK="ExternalOutput", YTK="ExternalOutput"):
    NBO = NBA // 2; TA = NBA * 128; NT = NBO * 128
    dt = lambda name, shape, d, kind="ExternalInput": nc.dram_tensor(name, shape, d, kind=kind).ap()
    return dict(
        xall=dt("xall", [TA, 2048], F32), wu=dt("wu", [8, 128, 16, 128], F32), wq=dt("wq", [8, 128, 16, 128], F32),
        wk=dt("wk", [8, 128, 16, 128], F32), wvs=dt("wvs", [2, 128, 16, 512], F32), wv=dt("wv", [2, 128, 16, 512], F32),
        wf=dt("wf", [128, 16, 8], F32), bau=dt("bau", [128, 8], F32), bav=dt("bav", [1024], F32),
        gv=dt("gv", [1024], F32), bv=dt("bv", [1024], F32), wsT=dt("wsT", [128, 8, 128], F32),
        bs=dt("bs", [1024], F32), bf=dt("bf", [8], F32), hf=dt("hf", [128, 2], F32),
        maskT=dt("maskT", [NBO, 128, NBA, 128], BF16),
        uTd=dt("uTd", [128, 8, NT], BF16, SCRK), qTd=dt("qTd", [128, 8, NT], BF16, SCRK),
        vsd=dt("vsd", [NT, 1024], BF16, SCRK), kT=dt("kT", [128, 8, TA], BF16, SCRK),
        V=dt("V", [TA, 1024], BF16, SCRK), yT=dt("yT", [128, 16, NT], BF16, YTK),
    )


def front0_host(x_true, h, w_in, b_a, w_s, b_s, g_v, b_v, b_f, NBA):
    NBO = NBA // 2; TA = NBA * 128; NT = NBO * 128
    xall = np.concatenate([x_true[h * NT:(h + 1) * NT], x_true[(1 - h) * NT:(2 - h) * NT]], 0)
    r = lambda a: np.ascontiguousarray(a.astype(np.float32))
    fm = lambda W: r(W.reshape(16, 128, -1, 128).transpose(2, 1, 0, 3))
    tm = lambda W: r(W.reshape(16, 128, -1, 512).transpose(2, 1, 0, 3))
    m = np.arange(TA); tp = (m + h * NT) % TA
    qtrue = tp[:NT].reshape(NBO, 1, 1, 128)
    ktrue = tp.reshape(NBA, 128).T.reshape(1, 128, NBA, 1)
    maskT = np.where(ktrue <= qtrue, 0.0, NEG).astype(ml_dtypes.bfloat16)
    return dict(
        xall=r(xall), wu=fm(w_in[:, 0:1024]), wq=fm(w_in[:, 2048:3072]), wk=fm(w_in[:, 3072:4096]),
        wvs=tm(w_in[:, 1024:2048]), wv=tm(w_in[:, 4096:5120]), wf=r(w_in[:, 5120:5128].reshape(16, 128, 8).transpose(1, 0, 2)),
        bau=r(b_a[:1024].reshape(8, 128).T), bav=r(b_a[1024:]), gv=r(g_v), bv=r(b_v),
        wsT=r(w_s.transpose(2, 0, 1)), bs=r(b_s.reshape(-1)), bf=r(b_f),
        hf=r(np.tile(np.array([[h, 1 - h]], np.float32), (128, 1))), maskT=np.ascontiguousarray(maskT))


def rwkv_drams(nc, T, NCT):
    NCHK = T // 64; NH = 2 * NCT
    dt = lambda name, shape, d, kind="ExternalInput": nc.dram_tensor(name, shape, d, kind=kind).ap()
    O = os.environ.get("SCR", "Internal")
    d = dict(
        x=dt("x", [T, 2048], F32), wrkv=dt("wrkv", [3, NCT, 128, 16, 128], F32), w1=dt("w1", [128, 16, 96], F32),
        a1=dt("a1", [128, 16, 96], F32), g1=dt("g1", [128, 16, 256], F32), w2=dt("w2", [96, NCT * 128], F32),
        a2=dt("a2", [96, NCT * 128], F32), g2=dt("g2", [128, 2, NCT * 128], F32), mu=dt("mu", [128, 6, 16], F32),
        ARt=dt("ARt", [128, NCT, NCHK, 128], BF16, O), wend=dt("wend", [128, NCT, NCHK], F32, O),
        gT=dt("gT", [128, NCT, T], BF16, O), GT=dt("GT", [NCHK, 64, NH, 64], BF16, O), RpT=dt("RpT", [NCHK, 64, NH, 64], BF16, O),
        Y0p=dt("Y0p", [NCHK, 64, NH, 64], F32, O), Hm=dt("Hm", [NCHK, 64, NH, 64], F32, O),
        ygT=dt("ygT", [128, NCT, T], BF16, "ExternalOutput"),
    )
    for nm in ("w0", "a0", "kk", "ka", "rk", "gng", "gnb"):
        d[nm] = dt(nm, [128, NCT], F32)
    for nm in ("Bt", "Kt", "Bht", "Kht", "vt"):
        d[nm] = dt(nm, [128, NCT, T], BF16, O)
    for nm in ("r32", "k32", "v32", "y32"):
        d[nm] = dt(nm, [128, NCT, T], F32, O)
    return d


def rwkv_host(x, p, ct0, NCT):
    r = lambda a: np.ascontiguousarray(np.asarray(a, np.float32))
    c0, c1 = ct0 * 128, (ct0 + NCT) * 128
    fm = lambda W: r(W.reshape(16, 128, -1).transpose(1, 0, 2))
    wrkv = np.stack([p["w_rkv"][i][:, c0:c1].reshape(16, 128, NCT, 128).transpose(2, 1, 0, 3) for i in range(3)], 0)
    col = lambda v: r(np.asarray(v).reshape(-1)[c0:c1].reshape(NCT, 128).T)
    return dict(
        x=r(x), wrkv=r(wrkv), w1=fm(p["w1"]), a1=fm(p["a1"]), g1=fm(p["g1"]), w2=r(p["w2"][:, c0:c1]), a2=r(p["a2"][:, c0:c1]),
        g2=r(p["g2"][:, c0:c1].reshape(2, 128, NCT * 128).transpose(1, 0, 2)), mu=r(p["mu"].reshape(6, 16, 128).transpose(2, 0, 1)),
        w0=col(p["w0"]), a0=col(p["a0"]), kk=col(p["k_k"]), ka=col(p["k_a"]), rk=col(p["r_k"]), gng=col(p["gn_g"]), gnb=col(p["gn_b"]))


def _tail_drams(nc, NT, with_y):
    dt = lambda name, shape, d, kind="ExternalInput": nc.dram_tensor(name, shape, d, kind=kind).ap()
    d = dict(
        wo=dt("wo", [128, 16, 2048], F32), lng=dt("lng", [2, 2048], F32), lnb=dt("lnb", [2, 2048], F32),
        wr=dt("wr", [128, 16, 16], F32), br=dt("br", [16], F32), wgu=dt("wgu", [16, 8, 128, 16, 2, 128], F32),
        wd=dt("wd", [16, 4, 128, 8, 512], F32), x1s=dt("x1s", [NT, 2048], F32, "Internal"),
        x1T=dt("x1T", [128, 16, NT], BF16, "Internal"), out=dt("out", [NT, 2048], F32, "ExternalOutput"))
    if with_y:
        d["yT"] = dt("yT", [128, 16, NT], BF16)
        d["xres"] = dt("xres", [NT, 2048], F32)
    return d


def _build_L1():
    nc = bass.Bass("TRN2", target_bir_lowering=False)
    NBA, NT = 32, 2048
    dr = front0_drams(nc, NBA, "Internal", "Internal")
    dr.update(_tail_drams(nc, NT, False))
    dr["xres"] = dr["xall"][0:NT, :]
    S = Sched(nc)
    C = Ctx(nc)
    with ExitStack() as st:
        identf, identb = make_ident(S, C, st)
        emit_front0(S, C, nc, NBA, dr, identf, identb)
        emit_tail(S, C, nc, NT, 1024, dr, 0, identf, identb)
        S.finalize([("out", b) for b in range(NT // 128)])
    return nc


def _build_L2():
    nc = bass.Bass("TRN2", target_bir_lowering=False)
    dr = rwkv_drams(nc, 4096, 8)
    S = Sched(nc)
    C = Ctx(nc)
    with ExitStack() as st:
        identf, identb = make_ident(S, C, st)
        emit_rwkv(S, C, nc, 4096, 8, dr, identf, identb)
        S.finalize([("ygT", ct, tt) for ct in range(8) for tt in range(8)])
    return nc


def _build_L3():
    nc = bass.Bass("TRN2", target_bir_lowering=False)
    NT = 2048
    dr = _tail_drams(nc, NT, True)
    S = Sched(nc)
    C = Ctx(nc)
    with ExitStack() as st:
        identf, identb = make_ident(S, C, st)
        emit_tail(S, C, nc, NT, 1024, dr, 1, identf, identb)
        S.finalize([("out", b) for b in range(NT // 128)])
    return nc


def _tail_host(W, lng, lnb, w_router, b_router, w_gu, w_down):
    r = lambda a: np.ascontiguousarray(np.asarray(a, np.float32))
    return dict(
        wo=r(np.asarray(W).reshape(16, 128, 2048).transpose(1, 0, 2)), lng=r(lng), lnb=r(lnb),
        wr=r(np.asarray(w_router).reshape(16, 128, 16).transpose(1, 0, 2)), br=r(b_router),
        wgu=r(np.asarray(w_gu).reshape(16, 16, 128, 2, 8, 128).transpose(0, 4, 2, 1, 3, 5)),
        wd=r(np.asarray(w_down).reshape(16, 8, 128, 4, 512).transpose(0, 3, 2, 1, 4)))


def kernel(x, ev_w_in, ev_b_a, ev_w_s, ev_b_s, ev_g_v, ev_b_v, ev_b_f, ev_w_out,
           rw_mu, rw_w_rkv, rw_w0, rw_w1, rw_w2, rw_a0, rw_a1, rw_a2, rw_g1, rw_g2,
           rw_k_k, rw_k_a, rw_r_k, rw_gn_g, rw_gn_b, rw_w_o,
           ln_g, ln_b, w_router, b_router, w_gu, w_down):
    n = 8
    NT = 2048
    x = np.asarray(x, np.float32)
    A = lambda a: np.asarray(a, np.float32)
    t0 = _tail_host(ev_w_out[0], ln_g[0], ln_b[0], w_router, b_router, w_gu[0], w_down[0])
    in_maps = []
    shared = None
    for c in range(n):
        b, h = c // 2, c % 2
        f = front0_host(x[b], h, A(ev_w_in[0]), A(ev_b_a[0]), A(ev_w_s[0]), A(ev_b_s[0]), A(ev_g_v[0]), A(ev_b_v[0]),
                        A(ev_b_f[0]), 32)
        if shared is None:
            shared = {k: v for k, v in f.items() if k not in ("xall", "hf", "maskT")}
        else:
            for k in shared:
                f[k] = shared[k]
        f.update(t0)
        in_maps.append(f)
    res = run_bass_kernel_spmd(_build_L1(), in_maps, core_ids=list(range(n)))
    x1 = np.stack([np.asarray(res.results[c]["out"], np.float32) for c in range(n)], 0).reshape(4, 4096, 2048)
    del res, in_maps, shared, t0
    p = dict(mu=A(rw_mu[0]), w_rkv=A(rw_w_rkv[0]), w0=A(rw_w0[0]), w1=A(rw_w1[0]), w2=A(rw_w2[0]), a0=A(rw_a0[0]),
             a1=A(rw_a1[0]), a2=A(rw_a2[0]), g1=A(rw_g1[0]), g2=A(rw_g2[0]), k_k=A(rw_k_k[0]), k_a=A(rw_k_a[0]),
             r_k=A(rw_r_k[0]), gn_g=A(rw_gn_g[0]), gn_b=A(rw_gn_b[0]))
    in_maps = [rwkv_host(x1[c // 2], p, (c % 2) * 8, 8) for c in range(n)]
    res = run_bass_kernel_spmd(_build_L2(), in_maps, core_ids=list(range(n)))
    yg = [np.asarray(res.results[c]["ygT"]) for c in range(n)]
    del res, in_maps
    t1 = _tail_host(rw_w_o[0], ln_g[1], ln_b[1], w_router, b_router, w_gu[1], w_down[1])
    in_maps = []
    for c in range(n):
        b, h = c // 2, c % 2
        yT = np.ascontiguousarray(np.concatenate([yg[2 * b][:, :, h * NT:(h + 1) * NT],
                                                  yg[2 * b + 1][:, :, h * NT:(h + 1) * NT]], axis=1))
        m = dict(t1)
        m["yT"] = yT
        m["xres"] = np.ascontiguousarray(x1[b, h * NT:(h + 1) * NT])
        in_maps.append(m)
    res = run_bass_kernel_spmd(_build_L3(), in_maps, core_ids=list(range(n)))
    out = np.stack([np.asarray(res.results[c]["out"], np.float32) for c in range(n)], 0).reshape(4, 4096, 2048)
    return out.astype(np.float32)
```
